# Optimizing a Trainium2 kernel written in Bass

```python
import math
import jax, jax.numpy as jnp
from jax import lax
import numpy as np

D_MODEL = 1024
BATCH = 8
SEQ = 2048
DEPTH = 2

CONV_CH = D_MODEL
CONV_K = 31
SSM_D_INNER = 2 * D_MODEL
SSM_HEADDIM = 64
SSM_HEADS = SSM_D_INNER // SSM_HEADDIM
SSM_GROUPS = 4
SSM_STATE = 128
SSM_CONV_K = 4
SSM_CHUNK = 128
SSM_XBC = SSM_D_INNER + 2 * SSM_GROUPS * SSM_STATE
N_BRANCHES = 2
IN_SPLIT = (CONV_CH, CONV_CH, SSM_D_INNER, SSM_XBC, SSM_HEADS, N_BRANCHES * D_MODEL)
D_IN_PROJ = 2 * CONV_CH + SSM_D_INNER + SSM_XBC + SSM_HEADS + N_BRANCHES * D_MODEL
FF_DENSE = 2816
N_EXPERTS = 8
TOP_K = 2
FF_EXPERT = 3584
N_DENSE = (DEPTH + 1) // 2
N_MOE = DEPTH // 2
LN_EPS = 1e-5
RMS_EPS = 1e-5
ALPHA = (2 * DEPTH) ** 0.25
BETA = (8 * DEPTH) ** -0.25

kernel_name = "hybrid_conformer_conv_mamba2_deepnorm_moe"


def layer_norm(x, g, b):
    xf = x.astype(jnp.float32)
    mu = jnp.mean(xf, -1, keepdims=True)
    var = jnp.mean(jnp.square(xf - mu), -1, keepdims=True)
    return ((xf - mu) * lax.rsqrt(var + LN_EPS) * g.astype(jnp.float32) + b.astype(jnp.float32)).astype(x.dtype)


def causal_depthwise_conv(u, w, b):
    k = w.shape[0]
    out = lax.conv_general_dilated(
        u, w[:, None, :], window_strides=(1,), padding=[(k - 1, 0)],
        dimension_numbers=("NWC", "WIO", "NWC"), feature_group_count=u.shape[-1])
    return out + b


def gated_group_rmsnorm(y, z, w):
    h = (y * jax.nn.silu(z)).astype(jnp.float32)
    hg = h.reshape(h.shape[:-1] + (SSM_GROUPS, SSM_D_INNER // SSM_GROUPS))
    hg = hg * lax.rsqrt(jnp.mean(jnp.square(hg), -1, keepdims=True) + RMS_EPS)
    return (hg.reshape(h.shape) * w.astype(jnp.float32)).astype(y.dtype)


def ssd_chunked(xs, dt, a, bm, cm):
    f32 = jnp.float32
    bsz, seqlen = xs.shape[0], xs.shape[1]
    nc = seqlen // SSM_CHUNK
    r = SSM_HEADS // SSM_GROUPS
    x = (xs.astype(f32) * dt[..., None]).reshape(bsz, nc, SSM_CHUNK, SSM_GROUPS, r, SSM_HEADDIM)
    da = (dt * a.astype(f32)).reshape(bsz, nc, SSM_CHUNK, SSM_GROUPS, r)
    a_cum = jnp.cumsum(jnp.moveaxis(da, 2, -1), axis=-1)
    bc = bm.astype(f32).reshape(bsz, nc, SSM_CHUNK, SSM_GROUPS, SSM_STATE)
    cc = cm.astype(f32).reshape(bsz, nc, SSM_CHUNK, SSM_GROUPS, SSM_STATE)
    pos = jnp.arange(SSM_CHUNK)
    causal = pos[:, None] >= pos[None, :]
    seg = a_cum[..., :, None] - a_cum[..., None, :]
    decay = jnp.exp(jnp.where(causal, seg, -jnp.inf))
    cb = jnp.einsum("bclgn,bcsgn->bcgls", cc, bc)
    y_diag = jnp.einsum("bcgls,bcgrls,bcsgrp->bclgrp", cb, decay, x)
    decay_to_end = jnp.exp(a_cum[..., -1:] - a_cum)
    states = jnp.einsum("bclgn,bcgrl,bclgrp->bcgrpn", bc, decay_to_end, x)
    chunk_decay = jnp.exp(a_cum[..., -1])

    def step(h, inp):
        s_c, d_c = inp
        return h * d_c[..., None, None] + s_c, h

    h0 = jnp.zeros((bsz, SSM_GROUPS, r, SSM_HEADDIM, SSM_STATE), f32)
    _, prev = lax.scan(step, h0, (jnp.moveaxis(states, 1, 0), jnp.moveaxis(chunk_decay, 1, 0)))
    prev = jnp.moveaxis(prev, 0, 1)
    y_off = jnp.einsum("bclgn,bcgrpn,bcgrl->bclgrp", cc, prev, jnp.exp(a_cum))
    return (y_diag + y_off).reshape(bsz, seqlen, SSM_HEADS, SSM_HEADDIM)


def hybrid_mixer(x, w_in, conv_dw_w, conv_dw_b, conv_ln_g, conv_ln_b, conv_w_out,
                 ssm_conv_w, ssm_conv_b, ssm_dt_bias, ssm_a_log, ssm_d, ssm_norm_w,
                 ssm_w_out, w_out):
    bsz, seqlen, _ = x.shape
    proj = x @ w_in
    cuts = np.cumsum(IN_SPLIT)[:-1].tolist()
    cv, cg, z, xbc, dt_raw, gates = jnp.split(proj, cuts, axis=-1)
    u = cv * jax.nn.sigmoid(cg)
    u = causal_depthwise_conv(u, conv_dw_w, conv_dw_b)
    u = jax.nn.silu(layer_norm(u, conv_ln_g, conv_ln_b))
    y_conv = u @ conv_w_out
    xbc = jax.nn.silu(causal_depthwise_conv(xbc, ssm_conv_w, ssm_conv_b))
    xs, bm, cm = jnp.split(xbc, [SSM_D_INNER, SSM_D_INNER + SSM_GROUPS * SSM_STATE], axis=-1)
    xs = xs.reshape(bsz, seqlen, SSM_HEADS, SSM_HEADDIM)
    bm = bm.reshape(bsz, seqlen, SSM_GROUPS, SSM_STATE)
    cm = cm.reshape(bsz, seqlen, SSM_GROUPS, SSM_STATE)
    dt = jax.nn.softplus((dt_raw + ssm_dt_bias).astype(jnp.float32))
    a = -jnp.exp(ssm_a_log.astype(jnp.float32))
    y = ssd_chunked(xs, dt, a, bm, cm)
    y = y + ssm_d.astype(jnp.float32)[:, None] * xs.astype(jnp.float32)
    y = y.reshape(bsz, seqlen, SSM_D_INNER).astype(x.dtype)
    y = gated_group_rmsnorm(y, z, ssm_norm_w)
    y_ssm = y @ ssm_w_out
    g_conv, g_ssm = jnp.split(gates, 2, axis=-1)
    h = jax.nn.sigmoid(g_conv) * y_conv + jax.nn.sigmoid(g_ssm) * y_ssm
    return h @ w_out


def swiglu(t, w_gate, w_up, w_down):
    return (jax.nn.silu(t @ w_gate) * (t @ w_up)) @ w_down


def moe_swiglu(x, w_router, w_gate, w_up, w_down):
    bsz, seqlen, d = x.shape
    t = x.reshape(bsz * seqlen, d)
    logits = (t @ w_router).astype(jnp.float32)
    top_vals, top_idx = lax.top_k(logits, TOP_K)
    top_w = jax.nn.softmax(top_vals, axis=-1)
    combine = jnp.sum(jax.nn.one_hot(top_idx, N_EXPERTS, dtype=jnp.float32) * top_w[..., None], axis=1)
    combine = combine.astype(x.dtype)
    y = jnp.zeros_like(t)
    for e in range(N_EXPERTS):
        y = y + combine[:, e:e + 1] * swiglu(t, w_gate[e], w_up[e], w_down[e])
    return y.reshape(bsz, seqlen, d)


def setup_inputs(seed: int = 0) -> dict:
    key = jax.random.key(seed)
    ks = iter(jax.random.split(key, 40))

    def nrm(shape, scale):
        return jax.random.normal(next(ks), shape, jnp.float32) * scale

    def near_one(shape):
        return 1.0 + nrm(shape, 0.02)

    dt_init = jnp.exp(jax.random.uniform(next(ks), (DEPTH, SSM_HEADS), jnp.float32,
                                         minval=math.log(1e-3), maxval=math.log(1e-1)))
    dt_bias = dt_init + jnp.log(-jnp.expm1(-dt_init))
    a_log = jnp.log(jax.random.uniform(next(ks), (DEPTH, SSM_HEADS), jnp.float32, minval=1.0, maxval=16.0))
    return {
        "x": nrm((BATCH, SEQ, D_MODEL), 1.0),
        "mix_w_in": nrm((DEPTH, D_MODEL, D_IN_PROJ), D_MODEL ** -0.5),
        "conv_dw_w": nrm((DEPTH, CONV_K, CONV_CH), CONV_K ** -0.5),
        "conv_dw_b": nrm((DEPTH, CONV_CH), 0.02),
        "conv_ln_g": near_one((DEPTH, CONV_CH)),
        "conv_ln_b": nrm((DEPTH, CONV_CH), 0.02),
        "conv_w_out": nrm((DEPTH, CONV_CH, D_MODEL), CONV_CH ** -0.5),
        "ssm_conv_w": nrm((DEPTH, SSM_CONV_K, SSM_XBC), SSM_CONV_K ** -0.5),
        "ssm_conv_b": nrm((DEPTH, SSM_XBC), 0.02),
        "ssm_dt_bias": dt_bias,
        "ssm_a_log": a_log,
        "ssm_d": near_one((DEPTH, SSM_HEADS)),
        "ssm_norm_w": near_one((DEPTH, SSM_D_INNER)),
        "ssm_w_out": nrm((DEPTH, SSM_D_INNER, D_MODEL), SSM_D_INNER ** -0.5),
        "mix_w_out": nrm((DEPTH, D_MODEL, D_MODEL), BETA * D_MODEL ** -0.5),
        "ln_mix_g": near_one((DEPTH, D_MODEL)),
        "ln_mix_b": nrm((DEPTH, D_MODEL), 0.02),
        "ffn_w_gate": nrm((N_DENSE, D_MODEL, FF_DENSE), D_MODEL ** -0.5),
        "ffn_w_up": nrm((N_DENSE, D_MODEL, FF_DENSE), D_MODEL ** -0.5),
        "ffn_w_down": nrm((N_DENSE, FF_DENSE, D_MODEL), BETA * FF_DENSE ** -0.5),
        "moe_router": nrm((N_MOE, D_MODEL, N_EXPERTS), D_MODEL ** -0.5),
        "moe_w_gate": nrm((N_MOE, N_EXPERTS, D_MODEL, FF_EXPERT), D_MODEL ** -0.5),
        "moe_w_up": nrm((N_MOE, N_EXPERTS, D_MODEL, FF_EXPERT), D_MODEL ** -0.5),
        "moe_w_down": nrm((N_MOE, N_EXPERTS, FF_EXPERT, D_MODEL), BETA * FF_EXPERT ** -0.5),
        "ln_ffn_g": near_one((DEPTH, D_MODEL)),
        "ln_ffn_b": nrm((DEPTH, D_MODEL), 0.02),
    }


def reference(x, mix_w_in, conv_dw_w, conv_dw_b, conv_ln_g, conv_ln_b, conv_w_out,
              ssm_conv_w, ssm_conv_b, ssm_dt_bias, ssm_a_log, ssm_d, ssm_norm_w, ssm_w_out,
              mix_w_out, ln_mix_g, ln_mix_b, ffn_w_gate, ffn_w_up, ffn_w_down,
              moe_router, moe_w_gate, moe_w_up, moe_w_down, ln_ffn_g, ln_ffn_b):
    for i in range(DEPTH):
        mix = hybrid_mixer(x, mix_w_in[i], conv_dw_w[i], conv_dw_b[i], conv_ln_g[i], conv_ln_b[i],
                           conv_w_out[i], ssm_conv_w[i], ssm_conv_b[i], ssm_dt_bias[i],
                           ssm_a_log[i], ssm_d[i], ssm_norm_w[i], ssm_w_out[i], mix_w_out[i])
        x = layer_norm(ALPHA * x + mix, ln_mix_g[i], ln_mix_b[i])
        j = i // 2
        if i % 2 == 0:
            f = swiglu(x, ffn_w_gate[j], ffn_w_up[j], ffn_w_down[j])
        else:
            f = moe_swiglu(x, moe_router[j], moe_w_gate[j], moe_w_up[j], moe_w_down[j])
        x = layer_norm(ALPHA * x + f, ln_ffn_g[i], ln_ffn_b[i])
    return x
```

```python
import numpy as np
import concourse.bass as bass
import concourse.mybir as mybir
from concourse.bass_utils import run_bass_kernel_spmd

F32 = mybir.dt.float32
BF16 = mybir.dt.bfloat16
U8 = mybir.dt.uint8
AF = mybir.ActivationFunctionType
ALU = mybir.AluOpType

L = 2048
D = 1024
NT = 16
H = 32
ALPHA = 4.0 ** 0.25
EPS = 1e-5
FF_DENSE = 2816
FF_EXP = 3584
NEXP = 8
NEXP_RUN = 8
STOP = None
FORCE_DENSE = False
ARENA = 204 * 1024


class Buf:
    def __init__(self, name, t):
        self.name = name
        self.t = t
        self.w = {}
        self.r = {}
        self.dsem = None

    def __getitem__(self, k):
        return self.t[k]


class Prog:
    ENG = ("pe", "act", "dve", "pool", "sp")

    def __init__(self, nc):
        self.nc = nc
        self.ops = {e: [] for e in self.ENG}
        self.sem_names = []
        self.sem_cnt = []
        self.esem = {}
        for e in self.ENG:
            self.esem[e] = self.new_sem("e_" + e)
        self.sem_eng = {v: k for k, v in self.esem.items()}
        self.last_op = {}
        self.seen = {e: {} for e in self.ENG}
        self._ctx = []
        cm = nc.sbuf_tensor("arena", [128, ARENA], U8)
        self.arena = cm.__enter__()
        self._ctx.append(cm)
        self.off = 0
        self.banks = []
        for i in range(8):
            cm = nc.psum_tensor("bank%d" % i, [128, 512], F32)
            self.banks.append(Buf("bank%d" % i, cm.__enter__()))
            self._ctx.append(cm)
        self.bank_i = 0
        self.n_instr = 0
        self.dsem_by_name = {}

    def new_sem(self, name):
        name = "%s_%d" % (name, len(self.sem_names))
        self.sem_names.append(name)
        self.sem_cnt.append(0)
        return len(self.sem_names) - 1

    def bank(self):
        b = self.banks[self.bank_i % 8]
        self.bank_i += 1
        return b

    def sb(self, name, shape, dt):
        esz = 4 if dt == F32 else 2
        n = 1
        for s in shape[1:]:
            n *= s
        off = (self.off + 63) // 64 * 64
        assert off + n * esz <= ARENA, ("SBUF arena overflow", name, off, n * esz)
        ap = self.arena[:, off:off + n * esz].bitcast(dt)
        if len(shape) == 3:
            ap = ap.rearrange("p (a b) -> p a b", b=shape[2])
        self.off = off + n * esz
        self.hw = max(getattr(self, "hw", 0), self.off)
        return Buf(name, ap)

    def mark(self):
        return self.off

    def release(self, m):
        self.barrier()
        self.hwlog = getattr(self, "hwlog", []) + [self.hw]
        self.hw = 0
        self.off = m

    def _collect(self, eng, reads, writes, partial):
        need = {}
        for b in reads:
            for s, v in b.w.items():
                if need.get(s, 0) < v:
                    need[s] = v
        for b in writes:
            if not partial:
                for s, v in b.w.items():
                    if need.get(s, 0) < v:
                        need[s] = v
            for s, v in b.r.items():
                if need.get(s, 0) < v:
                    need[s] = v
        waits = []
        seen = self.seen[eng]
        for s, v in need.items():
            if eng == "pe" and s == self.esem["pe"]:
                continue
            if seen.get(s, 0) >= v:
                continue
            self._materialize(s, v)
            seen[s] = v
            waits.append((s, v))
        return waits

    def _materialize(self, s, v):
        e = self.sem_eng.get(s)
        if e is None or v <= self.sem_cnt[s]:
            return
        assert v == self.sem_cnt[s] + 1
        ent = self.ops[e][self.last_op[e]]
        assert ent[2] is None
        ent[2] = (s, 1)
        self.sem_cnt[s] += 1

    def _commit(self, ev_s, ev_v, reads, writes, partial):
        for b in reads:
            if b.r.get(ev_s, 0) < ev_v:
                b.r[ev_s] = ev_v
        for b in writes:
            if partial:
                b.w[ev_s] = ev_v
            else:
                b.w = {ev_s: ev_v}
                b.r = {}

    def op(self, eng, fn, reads=(), writes=(), partial=False):
        waits = self._collect(eng, reads, writes, partial)
        s = self.esem[eng]
        self.ops[eng].append([waits, fn, None])
        self.last_op[eng] = len(self.ops[eng]) - 1
        self._commit(s, self.sem_cnt[s] + 1, reads, writes, partial)
        self.n_instr += 1

    def I(self, eng, name, reads, writes, partial=False, **kw):
        self.op(eng, lambda e, name=name, kw=kw: getattr(e, name)(**kw), reads, writes, partial)

    def mm(self, ob, out, lb, lhsT, rb, rhs, start, stop):
        self.op("pe", lambda e: e.matmul(out, lhsT, rhs, start=start, stop=stop), [lb, rb], [ob])

    def tr(self, ob, out, ib, in_, idb):
        self.op("pe", lambda e: e.transpose(out, in_, idb.t), [ib, idb], [ob])

    def dma(self, q, out_ap, in_ap, key, reads=(), writes=(), partial=False):
        if key.dsem is None:
            if key.name not in self.dsem_by_name:
                self.dsem_by_name[key.name] = self.new_sem("d_" + key.name)
            key.dsem = self.dsem_by_name[key.name]
        waits = self._collect(q, reads, writes, partial)
        s = key.dsem
        self.sem_cnt[s] += 16

        def fn(e):
            return e.dma_start(out=out_ap, in_=in_ap)
        self.ops[q].append([waits, fn, (s, 16)])
        self._commit(s, self.sem_cnt[s], reads, writes, partial)
        self.n_instr += 1

    def barrier(self):
        for e in self.ENG:
            li = self.last_op.get(e)
            if li is not None and self.ops[e][li][2] is None:
                self._materialize(self.esem[e], self.sem_cnt[self.esem[e]] + 1)
        allw = [(s, c) for s, c in enumerate(self.sem_cnt) if c > 0]
        for e in self.ENG:
            waits = []
            for s, v in allw:
                if self.seen[e].get(s, 0) >= v:
                    continue
                self.seen[e][s] = v
                waits.append((s, v))
            self.ops[e].append([waits, None, None])

    def emit(self):
        nc = self.nc
        sem_cms = [nc.semaphore(n) for n in self.sem_names]
        sems = [cm.__enter__() for cm in sem_cms]
        ops = self.ops

        def run(e, lst):
            for waits, fn, inc in lst:
                for s, v in waits:
                    e.wait_ge(sems[s], v)
                if fn is not None:
                    ins = fn(e)
                    if inc is not None:
                        ins.then_inc(sems[inc[0]], inc[1])

        with nc.Block() as block:
            @block.tensor
            def _(e):
                run(e, ops["pe"])

            @block.scalar
            def _(e):
                run(e, ops["act"])

            @block.vector
            def _(e):
                run(e, ops["dve"])

            @block.gpsimd
            def _(e):
                run(e, ops["pool"])

            @block.sync
            def _(e):
                run(e, ops["sp"])
        for cm in reversed(sem_cms):
            cm.__exit__(None, None, None)
        for cm in reversed(self._ctx):
            cm.__exit__(None, None, None)


def build(nc, nlayers=2, debug=False):
    P = Prog(nc)
    skind = "ExternalOutput" if debug else "Internal"

    def din(name, shape, dt=F32):
        return nc.dram_tensor(name, list(shape), dt, kind="ExternalInput").ap()

    def dscr(name, shape, dt):
        return P_dram(name, nc.dram_tensor(name, list(shape), dt, kind=skind).ap())

    def P_dram(name, ap):
        return Buf(name, ap)

    x_d = din("x", [L, D])
    out_d = nc.dram_tensor("out", [L, D], F32, kind="ExternalOutput").ap()
    c_identf = din("c_identf", [128, 128])
    c_U = din("c_U", [128, 128])
    c_Ls = din("c_Ls", [128, 128])
    c_ones = din("c_ones", [128, 128])
    c_idst = din("c_idst", [128, 64])
    Wd = []
    for l in range(2):
        w = {}
        w["win_a"] = din(f"win_a{l}", [14, 128, 8, 512])
        w["win_z"] = din(f"win_z{l}", [4, 128, 8, 512])
        w["win_dt"] = din(f"win_dt{l}", [128, 8, 32])
        w["cdw"] = din(f"cdw{l}", [128, 8, 31])
        w["cdb"] = din(f"cdb{l}", [128, 8])
        w["clg"] = din(f"clg{l}", [128, 8])
        w["clb"] = din(f"clb{l}", [128, 8])
        w["scw"] = din(f"scw{l}", [128, 24, 4])
        w["scb"] = din(f"scb{l}", [128, 24])
        w["dtb"] = din(f"dtb{l}", [128, 32])
        w["alog"] = din(f"alog{l}", [128, 32])
        w["dpp"] = din(f"dpp{l}", [128, 16])
        w["nw"] = din(f"nw{l}", [128, 16])
        w["cwo"] = din(f"cwo{l}", [128, 8, 1024])
        w["swo"] = din(f"swo{l}", [128, 16, 1024])
        w["wo"] = din(f"wo{l}", [128, 8, 1024])
        w["lmg"] = din(f"lmg{l}", [128, 1024])
        w["lmb"] = din(f"lmb{l}", [128, 1024])
        w["lfg"] = din(f"lfg{l}", [128, 1024])
        w["lfb"] = din(f"lfb{l}", [128, 1024])
        Wd.append(w)
    wgu0 = din("wgu0", [1, 22, 128, 8, 256])
    wd0 = din("wd0", [1, FF_DENSE, 1024])
    wgu1 = din("wgu1", [NEXP, 28, 128, 8, 256])
    wd1 = din("wd1", [NEXP, FF_EXP, 1024])
    wr_d = din("wr", [128, 8, 1024])

    identf = P.sb("identf", [128, 128], F32)
    identb = P.sb("identb", [128, 128], BF16)
    Um = P.sb("Um", [128, 128], F32)
    Ls = P.sb("Ls", [128, 128], F32)
    onesf = P.sb("onesf", [128, 128], F32)
    onesb = P.sb("onesb", [128, 128], BF16)
    idst = P.sb("idst", [128, 64], BF16)
    for b, d in ((identf, c_identf), (Um, c_U), (Ls, c_Ls), (onesf, c_ones)):
        P.dma("sp", b[:], d, b, writes=[b])
    P.dma("pool", identb[:], c_identf, identb, writes=[identb])
    P.dma("pool", onesb[:], c_ones, onesb, writes=[onesb])
    P.dma("pool", idst[:], c_idst, idst, writes=[idst])

    sp_ = {}
    for nm, shp in (("cdw", [128, 8, 31]), ("cdb", [128, 8]), ("clg", [128, 8]), ("clb", [128, 8]),
                    ("scw", [128, 24, 4]), ("scb", [128, 24]), ("dtb", [128, 32]), ("alog", [128, 32]),
                    ("dpp", [128, 16]), ("nw", [128, 16])):
        sp_[nm] = P.sb(nm, shp, F32)
    aneg = P.sb("aneg", [128, 32], F32)
    diagD = P.sb("diagD", [128, 16, 64], BF16)
    dtv = P.sb("dtv", [128, 16, 32], F32)
    dav = P.sb("dav", [128, 16, 32], F32)
    eA = P.sb("eA", [128, 16, 32], F32)
    eR = P.sb("eR", [128, 16, 32], F32)
    eT = P.sb("eT", [128, 16, 32], F32)
    dtR = P.sb("dtR", [128, 16, 32], F32)
    cwt = P.sb("cwt", [128, 16, 8], F32)
    lng = P.sb("lng", [128, 1024], F32)
    lnb = P.sb("lnb", [128, 1024], F32)
    epsb = P.sb("epsb", [128, 1], F32)
    P.I("dve", "memset", [], [epsb], ap=epsb[:], constant=EPS)

    def flat(ap3):
        return ap3.rearrange("p a b -> p (a b)")

    x_src = P_dram("x_in", x_d)

    for l in range(nlayers):
        w = Wd[l]
        last = (l == nlayers - 1)
        S_u = dscr(f"S_u{l}", [8, 128, L], BF16)
        S_xbc = dscr(f"S_xbc{l}", [24, 128, L], BF16)
        S_z = dscr(f"S_z{l}", [L, 2048], BF16)
        S_g = dscr(f"S_g{l}", [16, 128, L], BF16)
        S_yn = dscr(f"S_yn{l}", [16, 128, L], BF16)
        S_x1 = dscr(f"S_x1{l}", [L, D], F32)
        if last and nlayers == 2:
            x_dst = P_dram("out", out_d)
        elif last:
            x_dst = P_dram("out", out_d)
        else:
            x_dst = dscr(f"S_xl{l}", [L, D], F32)

        for nm in ("cdw", "cdb", "clg", "clb", "scw", "scb", "dtb", "alog", "dpp", "nw"):
            b = sp_[nm]
            P.dma("sp", b[:], w[nm], b, writes=[b])
        P.I("act", "activation", [sp_["alog"]], [aneg], out=aneg[:], in_=sp_["alog"][:], func=AF.Exp)
        P.I("dve", "tensor_scalar", [aneg], [aneg], out=aneg[:], in0=aneg[:], scalar1=-1.0, scalar2=None, op0=ALU.mult)
        for b_ in range(16):
            P.I("pool", "tensor_scalar", [idst, sp_["dpp"]], [diagD], partial=True, out=diagD[:, b_, :], in0=idst[:],
                scalar1=sp_["dpp"][:, b_:b_ + 1], scalar2=None, op0=ALU.mult)

        m_layer = P.mark()
        xT = P.sb("xT", [128, 8, L], BF16)

        mX = P.mark()
        xin = [P.sb(f"xin{i}", [128, 1024], BF16) for i in range(3)]
        for tt in range(NT):
            xt = xin[tt % 3]
            P.dma("pool", xt[:], x_src[tt * 128:(tt + 1) * 128, :], xt, reads=[x_src], writes=[xt])
            bk = P.bank()
            bkv = bk.t.bitcast(BF16)
            for k in range(8):
                P.tr(bk, bkv[:, k * 128:(k + 1) * 128], xt, xt[:, k * 128:(k + 1) * 128], identb)
            P.I("act", "copy", [bk], [xT], partial=True, out=xT[:, :, tt * 128:(tt + 1) * 128],
                in_=bkv[:, :].rearrange("p (j t) -> p j t", t=128))
        P.release(mX)

        mA = P.mark()
        wsl = [P.sb(f"wsl{i}", [128, 8, 512], BF16) for i in range(3)]
        sg = [P.sb(f"sg{i}", [128, L], BF16) for i in range(2)]
        upad = [P.sb(f"upad{i}", [128, 30 + L], BF16) for i in range(2)]
        dgw = [P.sb(f"dgw{i}", [128, 31, 128], BF16) for i in range(2)]
        uco = [P.sb(f"uco{i}", [128, L], BF16) for i in range(2)]
        xp = [P.sb(f"xp{i}", [128, 4 + L], F32) for i in range(2)]
        acc = [P.sb(f"acc{i}", [128, L], F32) for i in range(2)]
        xo = [P.sb(f"xo{i}", [128, L], BF16) for i in range(2)]
        go = [P.sb(f"go{i}", [128, L], BF16) for i in range(2)]
        for i in range(2):
            P.I("dve", "memset", [], [upad[i]], ap=upad[i][:, 0:30], constant=0.0)
            P.I("dve", "memset", [], [xp[i]], ap=xp[i][:, 0:4], constant=0.0)

        def load_slab(src, s, slot):
            P.dma("pool", flat(wsl[slot][:]), src[s].rearrange("p k n -> p (k n)"), wsl[slot], writes=[wsl[slot]])

        def conv31(i):
            up = upad[i % 2]
            dg = dgw[i % 2]
            uo = uco[i % 2]
            for t in range(4):
                bk = P.bank()
                for k in range(31):
                    P.mm(bk, bk[:], dg, dg[:, k, :], up, up[:, t * 512 + k:t * 512 + k + 512], k == 0, k == 30)
                P.I("act", "activation", [bk, sp_["cdb"]], [uo], partial=True, out=uo[:, t * 512:(t + 1) * 512], in_=bk[:],
                    func=AF.Identity, bias=sp_["cdb"][:, i:i + 1])
            P.dma("sp", S_u[i], uo[:], uo, reads=[uo], writes=[S_u], partial=True)

        load_slab(w["win_a"], 0, 0)
        load_slab(w["win_a"], 1, 1)
        pend_conv = None
        pend_silu = []
        for s in range(14):
            if s + 2 < 14:
                load_slab(w["win_a"], s + 2, (s + 2) % 3)
            ws = wsl[s % 3]
            for j in range(4):
                gb = s * 4 + j
                if gb < 16:
                    kind, idx = ("cg", gb // 2) if gb % 2 == 0 else ("cv", gb // 2)
                elif gb < 40:
                    kind, idx = "xbc", gb - 16
                else:
                    kind, idx = "gate", gb - 40
                if kind == "cg":
                    dg = dgw[idx % 2]
                    P.I("dve", "tensor_tensor", [identb, sp_["cdw"]], [dg], out=dg[:],
                        in0=identb[:].rearrange("p (o c) -> p o c", o=1).broadcast_to([128, 31, 128]),
                        in1=sp_["cdw"][:, idx, :].rearrange("p (k o) -> p k o", o=1).broadcast_to([128, 31, 128]), op=ALU.mult)
                for hf in range(2):
                    bks = [P.bank(), P.bank()]
                    for k in range(8):
                        for t in range(2):
                            c0 = hf * 1024 + t * 512
                            P.mm(bks[t], bks[t][:], ws, ws[:, k, j * 128:(j + 1) * 128], xT, xT[:, k, c0:c0 + 512], k == 0, k == 7)
                    for t in range(2):
                        c0 = hf * 1024 + t * 512
                        bk = bks[t]
                        if kind == "cg":
                            o = sg[idx % 2]
                            P.I("act", "activation", [bk], [o], partial=True, out=o[:, c0:c0 + 512], in_=bk[:], func=AF.Sigmoid)
                        elif kind == "cv":
                            o = upad[idx % 2]
                            P.I("dve", "tensor_tensor", [bk, sg[idx % 2]], [o], partial=True, out=o[:, 30 + c0:30 + c0 + 512],
                                in0=bk[:], in1=sg[idx % 2][:, c0:c0 + 512], op=ALU.mult)
                        elif kind == "xbc":
                            o = xp[idx % 2]
                            P.I("act", "copy", [bk], [o], partial=True, out=o[:, 3 + c0:3 + c0 + 512], in_=bk[:])
                        else:
                            o = go[idx % 2]
                            P.I("act", "activation", [bk], [o], partial=True, out=o[:, c0:c0 + 512], in_=bk[:], func=AF.Sigmoid)
                if kind == "cv":
                    if pend_conv is not None:
                        conv31(pend_conv)
                    pend_conv = idx
                elif kind == "xbc":
                    if pend_conv is not None:
                        conv31(pend_conv)
                        pend_conv = None
                    xpb = xp[idx % 2]
                    ac = acc[idx % 2]
                    P.I("act", "activation", [xpb, sp_["scw"], sp_["scb"]], [ac], out=ac[:], in_=xpb[:, 0:L], func=AF.Identity,
                        scale=sp_["scw"][:, idx, 0:1], bias=sp_["scb"][:, idx:idx + 1])
                    while pend_silu:
                        pend_silu.pop(0)()
                    for k in range(1, 4):
                        P.I("dve", "scalar_tensor_tensor", [xpb, sp_["scw"], ac], [ac], out=ac[:], in0=xpb[:, k:k + L],
                            scalar=sp_["scw"][:, idx, k:k + 1], in1=ac[:], op0=ALU.mult, op1=ALU.add)
                    def silu_out(idx=idx, ac=ac):
                        o = xo[idx % 2]
                        P.I("act", "activation", [ac], [o], out=o[:], in_=ac[:], func=AF.Silu)
                        P.dma("sp", S_xbc[idx], o[:], o, reads=[o], writes=[S_xbc], partial=True)
                    pend_silu.append(silu_out)
                elif kind == "gate":
                    while pend_silu:
                        pend_silu.pop(0)()
                    o = go[idx % 2]
                    P.dma("sp", S_g[idx], o[:], o, reads=[o], writes=[S_g], partial=True)
        P.release(mA)

        mA2 = P.mark()
        wsl = [P.sb(f"wslz{i}", [128, 8, 512], BF16) for i in range(3)]
        wdt = P.sb("wdt", [128, 8, 32], BF16)
        zo = [P.sb(f"zo{i}", [128, 512], BF16) for i in range(3)]
        P.dma("pool", wdt[:], w["win_dt"], wdt, writes=[wdt])
        load_slab(w["win_z"], 0, 0)
        load_slab(w["win_z"], 1, 1)
        for tt in range(NT):
            bk = P.bank()
            for k in range(8):
                P.mm(bk, bk[:, 0:32], xT, xT[:, k, tt * 128:(tt + 1) * 128], wdt, wdt[:, k, :], k == 0, k == 7)
            P.I("dve", "tensor_tensor", [bk, sp_["dtb"]], [dtv], partial=True, out=dtv[:, tt, :], in0=bk[:, 0:32],
                in1=sp_["dtb"][:], op=ALU.add)
        tmpa = P.sb("tmpa", [128, 512], F32)
        tmpb = P.sb("tmpb", [128, 512], F32)
        dtf = flat(dtv[:])
        P.I("dve", "tensor_scalar", [dtv], [tmpb], out=tmpb[:], in0=dtf, scalar1=0.0, scalar2=None, op0=ALU.max)
        P.I("dve", "tensor_scalar", [dtv], [tmpa], out=tmpa[:], in0=dtf, scalar1=0.0, scalar2=None, op0=ALU.min)
        P.I("dve", "tensor_tensor", [tmpa, tmpb], [tmpa], out=tmpa[:], in0=tmpa[:], in1=tmpb[:], op=ALU.subtract)
        P.I("act", "activation", [tmpa], [tmpa], out=tmpa[:], in_=tmpa[:], func=AF.Exp)
        P.I("act", "activation", [tmpa], [tmpa], out=tmpa[:], in_=tmpa[:], func=AF.Ln, bias=1.0)
        P.I("dve", "tensor_tensor", [tmpa, tmpb], [dtv], out=dtf, in0=tmpa[:], in1=tmpb[:], op=ALU.add)
        P.I("dve", "tensor_tensor", [dtv, aneg], [dav], out=dav[:], in0=dtv[:],
            in1=aneg[:].rearrange("p (o h) -> p o h", o=1).broadcast_to([128, 16, 32]), op=ALU.mult)
        for (lhs, dst) in ((Um, eA), (Ls, eR), (onesf, eT)):
            bk = P.bank()
            P.mm(bk, bk[:], lhs, lhs[:], dav, flat(dav[:]), True, True)
            P.I("act", "activation", [bk], [dst], out=flat(dst[:]), in_=bk[:], func=AF.Exp)
        P.I("dve", "tensor_tensor", [dtv, eR], [dtR], out=dtR[:], in0=dtv[:], in1=eR[:], op=ALU.mult)
        for s in range(4):
            if s + 2 < 4:
                load_slab(w["win_z"], s + 2, (s + 2) % 3)
            ws = wsl[s % 3]
            for tt in range(NT):
                bk = P.bank()
                for k in range(8):
                    P.mm(bk, bk[:], xT, xT[:, k, tt * 128:(tt + 1) * 128], ws, ws[:, k, :], k == 0, k == 7)
                o = zo[(s * NT + tt) % 3]
                P.I("act", "activation", [bk], [o], out=o[:], in_=bk[:], func=AF.Silu)
                P.dma("sp", S_z[tt * 128:(tt + 1) * 128, s * 512:(s + 1) * 512], o[:], o, reads=[o], writes=[S_z], partial=True)
        P.release(mA2)
        P.release(m_layer)
        if STOP == (l, "A2"):
            P.barrier()
            P.emit()
            return P

        mBC = P.mark()
        cwo = P.sb("cwo", [128, 8, 1024], BF16)
        wo = P.sb("wo", [128, 8, 1024], BF16)
        P.dma("pool", flat(cwo[:]), w["cwo"].rearrange("p k n -> p (k n)"), cwo, writes=[cwo])
        P.dma("pool", flat(wo[:]), w["wo"].rearrange("p k n -> p (k n)"), wo, writes=[wo])
        mB = P.mark()
        xb2 = [P.sb(f"xb{i}", [128, 24, 256], BF16) for i in range(2)]
        zt = [P.sb(f"zt{i}", [128, 2048], BF16) for i in range(2)]
        xdt2 = [P.sb(f"xdt{i}", [128, 2048], BF16) for i in range(2)]
        xdec2 = [P.sb(f"xdec{i}", [128, 2048], BF16) for i in range(2)]
        btok2 = [P.sb(f"btok{i}", [128, 512], BF16) for i in range(2)]
        MT2 = [P.sb(f"MT{i}", [128, 32, 128], BF16) for i in range(2)]
        rsg = P.sb("rsg", [128, 32, 128], F32)
        Eb = P.sb("Eb", [128, 32, 128], BF16)
        cbm = P.sb("cbm", [128, 4, 128], BF16)
        St = P.sb("St", [128, 2048], F32)
        Sbf = P.sb("Sbf", [128, 2048], BF16)
        t1 = [P.sb(f"t1{i}", [128, 512], F32) for i in range(2)]
        yv = [P.sb(f"yv{i}", [128, 512], F32) for i in range(2)]
        hg = P.sb("hg", [128, 2048], F32)
        junk = P.sb("junk", [128, 512], BF16)
        ss = P.sb("ss", [128, 4], F32)
        rs4 = P.sb("rs4", [128, 4], F32)
        hn = P.sb("hn", [128, 2048], BF16)
        yno = [P.sb(f"yno{i}", [128, 16, 128], BF16) for i in range(2)]
        P.I("dve", "memset", [], [St], ap=St[:], constant=0.0)
        P.I("dve", "memset", [], [Sbf], ap=Sbf[:], constant=0.0)
        bT0, bTh, bT2, bSeg0, bSeg1, bY, bYo, bSn = P.banks

        def bfv(bk):
            return bk.t.bitcast(BF16)

        def ssd_front(c):
            cc = c % 2
            xb = xb2[(c // 2) % 2]
            xdt, xdec, btok, MT = xdt2[c % 2], xdec2[c % 2], btok2[c % 2], MT2[c % 2]
            def load_pair(pr):
                xbp = xb2[pr % 2]
                for q4 in range(4):
                    P.dma("sp", xbp[:, q4 * 6:(q4 + 1) * 6, :],
                          S_xbc[q4 * 6:(q4 + 1) * 6, :, pr * 256:pr * 256 + 256].rearrange("b p t -> p b t"),
                          xbp, reads=[S_xbc], writes=[xbp], partial=(q4 > 0))
            if c == 0:
                load_pair(0)
            if cc == 1 and c + 1 < NT:
                load_pair((c + 1) // 2)
            z_ = zt[c % 2]
            P.dma("sp", z_[:], S_z[c * 128:(c + 1) * 128, :], z_, reads=[S_z], writes=[z_])
            cs = slice(cc * 128, (cc + 1) * 128)
            P.I("dve", "tensor_tensor", [Um, dav], [rsg], out=rsg[:],
                in0=Um[:].rearrange("p (o l) -> p o l", o=1).broadcast_to([128, 32, 128]),
                in1=dav[:, c, :].rearrange("p (h o) -> p h o", o=1).broadcast_to([128, 32, 128]), op=ALU.mult)
            for g in range(4):
                P.tr(bT2, bfv(bT2)[:, g * 128:(g + 1) * 128], xb, xb[:, 16 + g, cs], identb)
            P.I("act", "copy", [bT2], [btok], out=btok[:], in_=bfv(bT2)[:, 0:512])
            for g in range(4):
                P.mm(bT2, bT2[:, g * 128:(g + 1) * 128], xb, xb[:, 16 + g, cs], xb, xb[:, 20 + g, cs], True, True)
            P.I("dve", "tensor_tensor", [bT2, Um], [cbm], out=cbm[:], in0=bT2[:].rearrange("p (g l) -> p g l", l=128),
                in1=Um[:].rearrange("p (o l) -> p o l", o=1).broadcast_to([128, 4, 128]), op=ALU.mult)
            for i_ in range(2):
                for b_ in range(8):
                    P.tr(bT0, bfv(bT0)[:, b_ * 128:(b_ + 1) * 128], xb, xb[:, i_ * 8 + b_, cs], identb)
                hs = slice(i_ * 16, (i_ + 1) * 16)
                for (dst, sc) in ((xdt, dtv), (xdec, dtR)):
                    P.I("dve", "tensor_tensor", [bT0, sc], [dst], partial=(i_ > 0),
                        out=dst[:, i_ * 1024:(i_ + 1) * 1024].rearrange("p (h q) -> p h q", q=64),
                        in0=bfv(bT0)[:, :].rearrange("p (h q) -> p h q", q=64),
                        in1=sc[:, c, hs].rearrange("p (h o) -> p h o", o=1).broadcast_to([128, 16, 64]), op=ALU.mult)

        def ssd_seg(c, q):
            MT = MT2[c % 2]
            bk = bSeg0 if q % 2 == 0 else bSeg1
            P.mm(bk, bk[:], Ls, Ls[:], rsg, flat(rsg[:])[:, q * 512:(q + 1) * 512], True, True)
            P.I("act", "activation", [bk], [Eb], partial=(q > 0), out=flat(Eb[:])[:, q * 512:(q + 1) * 512], in_=bk[:], func=AF.Exp)
            if q % 2 == 1:
                g = q // 2
                P.I("dve", "tensor_tensor", [Eb, cbm], [MT], partial=(g > 0), out=MT[:, g * 8:(g + 1) * 8, :], in0=Eb[:, g * 8:(g + 1) * 8, :],
                    in1=cbm[:, g:g + 1, :].broadcast_to([128, 8, 128]), op=ALU.mult)

        def ssd_back(c, nxt):
            cc = c % 2
            xb = xb2[(c // 2) % 2]
            xdt, xdec, btok, MT = xdt2[c % 2], xdec2[c % 2], btok2[c % 2], MT2[c % 2]
            z_ = zt[c % 2]
            cs = slice(cc * 128, (cc + 1) * 128)
            P.I("dve", "memset", [], [ss], ap=ss[:], constant=0.0)
            for g in range(4):
                for r in range(8):
                    h = g * 8 + r
                    blk = h // 2
                    po = (h % 2) * 64
                    P.mm(bY, bY[:, r * 64:(r + 1) * 64], xb, xb[po:po + 64, blk, cs], diagD, diagD[po:po + 64, blk, :], True, False)
                    P.mm(bY, bY[:, r * 64:(r + 1) * 64], MT, MT[:, h, :], xdt, xdt[:, h * 64:(h + 1) * 64], False, True)
                if c > 0:
                    P.mm(bYo, bYo[:], xb, xb[:, 20 + g, cs], Sbf, Sbf[:, g * 512:(g + 1) * 512], True, True)
                if c < NT - 1:
                    P.mm(bSn, bSn[:], btok, btok[:, g * 128:(g + 1) * 128], xdec, xdec[:, g * 512:(g + 1) * 512], True, True)
                y_ = yv[g % 2]
                if c > 0:
                    t_ = t1[g % 2]
                    P.I("dve", "tensor_tensor", [bYo, eA], [t_], out=t_[:].rearrange("p (r q) -> p r q", q=64),
                        in0=bYo[:].rearrange("p (r q) -> p r q", q=64),
                        in1=eA[:, c, g * 8:(g + 1) * 8].rearrange("p (r o) -> p r o", o=1).broadcast_to([128, 8, 64]), op=ALU.mult)
                    P.I("dve", "tensor_tensor", [bY, t_], [y_], out=y_[:], in0=bY[:], in1=t_[:], op=ALU.add)
                else:
                    P.I("act", "copy", [bY], [y_], out=y_[:], in_=bY[:])
                P.I("dve", "tensor_tensor", [y_, z_], [hg], partial=(g > 0), out=hg[:, g * 512:(g + 1) * 512], in0=y_[:],
                    in1=z_[:, g * 512:(g + 1) * 512], op=ALU.mult)
                P.I("act", "activation", [hg], [junk, ss], out=junk[:], in_=hg[:, g * 512:(g + 1) * 512], func=AF.Square,
                    accum_out=ss[:, g:g + 1])
                if c < NT - 1:
                    sl = slice(g * 512, (g + 1) * 512)
                    P.I("dve", "tensor_tensor", [St, eT], [St], out=St[:, sl].rearrange("p (r q) -> p r q", q=64),
                        in0=St[:, sl].rearrange("p (r q) -> p r q", q=64),
                        in1=eT[:, c, g * 8:(g + 1) * 8].rearrange("p (r o) -> p r o", o=1).broadcast_to([128, 8, 64]), op=ALU.mult)
                    P.I("dve", "tensor_tensor", [St, bSn], [St], out=St[:, sl], in0=St[:, sl], in1=bSn[:], op=ALU.add)
                    P.I("act", "copy", [St], [Sbf], out=Sbf[:, sl], in_=St[:, sl])
                if nxt is not None:
                    ssd_seg(nxt, 2 * g)
                    ssd_seg(nxt, 2 * g + 1)
            P.I("act", "activation", [ss, epsb], [rs4], out=rs4[:], in_=ss[:], func=AF.Ln, scale=1.0 / 512.0, bias=epsb[:])
            P.I("act", "activation", [rs4], [rs4], out=rs4[:], in_=rs4[:], func=AF.Exp, scale=-0.5)
            for g in range(4):
                P.I("act", "activation", [hg, rs4], [hn], partial=(g > 0), out=hn[:, g * 512:(g + 1) * 512], in_=hg[:, g * 512:(g + 1) * 512],
                    func=AF.Identity, scale=rs4[:, g:g + 1])

        def ssd_tail(c):
            yo = yno[c % 2]
            for i_ in range(2):
                for b_ in range(8):
                    P.tr(bTh, bfv(bTh)[:, b_ * 128:(b_ + 1) * 128], hn, hn[:, (i_ * 8 + b_) * 128:(i_ * 8 + b_ + 1) * 128], identb)
                P.I("act", "copy", [bTh], [yo], partial=(i_ > 0), out=yo[:, i_ * 8:(i_ + 1) * 8, :],
                    in_=bfv(bTh)[:, :].rearrange("p (b t) -> p b t", t=128))
            P.dma("sp", S_yn[:, :, c * 128:(c + 1) * 128].rearrange("b p t -> p b t"), yo[:], yo, reads=[yo], writes=[S_yn], partial=True)

        ssd_front(0)
        for q in range(8):
            ssd_seg(0, q)
        for c in range(NT):
            if c + 1 < NT:
                ssd_front(c + 1)
            if c > 0:
                ssd_tail(c - 1)
            ssd_back(c, c + 1 if c + 1 < NT else None)
        ssd_tail(NT - 1)
        P.release(mB)
        if STOP == (l, "B"):
            P.barrier()
            P.emit()
            return P

        mC = P.mark()
        swo = P.sb("swo", [128, 16, 1024], BF16)
        P.dma("pool", flat(swo[:]), w["swo"].rearrange("p k n -> p (k n)"), swo, writes=[swo])
        for q2 in range(2):
            P.I("dve", "tensor_tensor", [swo, sp_["nw"]], [swo], partial=(q2 > 0), out=swo[:, q2 * 8:(q2 + 1) * 8, :], in0=swo[:, q2 * 8:(q2 + 1) * 8, :],
                in1=sp_["nw"][:, q2 * 8:(q2 + 1) * 8].rearrange("p (k o) -> p k o", o=1).broadcast_to([128, 8, 1024]), op=ALU.mult)
        P.dma("sp", lng[:], w["lmg"], lng, writes=[lng])
        P.dma("sp", lnb[:], w["lmb"], lnb, writes=[lnb])
        ucin = P.sb("ucin", [128, 8, 512], BF16)
        ucb = P.sb("ucb", [128, 8, 512], BF16)
        ynin = P.sb("ynin", [128, 16, 512], BF16)
        gin = [P.sb(f"gin{i}", [128, 2, 512], BF16) for i in range(2)]
        hT = P.sb("hT", [128, 8, 512], BF16)
        tk = [P.sb(f"tk{i}", [128, 512], F32) for i in range(2)]
        ta = [P.sb(f"ta{i}", [128, 512], F32) for i in range(2)]
        tb = [P.sb(f"tb{i}", [128, 512], F32) for i in range(2)]
        mean = P.sb("mean", [128, 512], F32)
        msq = P.sb("msq", [128, 512], F32)
        Av = P.sb("Av", [128, 512], F32)
        Bv = P.sb("Bv", [128, 512], F32)
        xres = [P.sb(f"xres{i}", [128, 1024], F32) for i in range(2)]
        vv4 = P.sb("vv4", [128, 4, 1024], F32)
        mv4 = P.sb("mv4", [128, 4, 2], F32)
        rs4c = P.sb("rs4c", [128, 4], F32)
        nm4 = P.sb("nm4", [128, 4], F32)
        xn = [P.sb(f"xn{i}", [128, 1024], F32) for i in range(2)]
        st6 = P.sb("st6", [128, 2, 6], F32)
        mv = P.sb("mv", [128, 2], F32)
        rstd = P.sb("rstd", [128, 1], F32)
        nmr = P.sb("nmr", [128, 1], F32)

        def layer_norm_rows(v, o, stq):
            for i_ in range(2):
                P.I("dve", "bn_stats", [v], [stq], partial=(i_ > 0), out=stq[:, i_, :], in_=v[:, i_ * 512:(i_ + 1) * 512])
            P.I("dve", "bn_aggr", [stq], [mv], out=mv[:], in_=stq[:])
            P.I("act", "activation", [mv, epsb], [rstd], out=rstd[:], in_=mv[:, 1:2], func=AF.Sqrt, bias=epsb[:])
            P.I("dve", "reciprocal", [rstd], [rstd], out=rstd[:], in_=rstd[:])
            P.I("dve", "scalar_tensor_tensor", [mv, rstd], [nmr], out=nmr[:], in0=mv[:, 0:1], scalar=-1.0, in1=rstd[:],
                op0=ALU.mult, op1=ALU.mult)
            P.I("act", "activation", [v, rstd, nmr], [o], out=o[:], in_=v[:], func=AF.Identity, scale=rstd[:], bias=nmr[:])
            P.I("dve", "tensor_tensor", [o, lng], [o], out=o[:], in0=o[:], in1=lng[:], op=ALU.mult)
            P.I("dve", "tensor_tensor", [o, lnb], [o], out=o[:], in0=o[:], in1=lnb[:], op=ALU.add)

        def c_prep(T):
            ts_ = slice(T * 512, (T + 1) * 512)
            P.dma("sp", ucin[:], S_u[:, :, ts_].rearrange("b p t -> p b t"), ucin, reads=[S_u], writes=[ucin])
            for q2 in range(2):
                P.dma("sp", ynin[:, q2 * 8:(q2 + 1) * 8, :], S_yn[q2 * 8:(q2 + 1) * 8, :, ts_].rearrange("b p t -> p b t"), ynin,
                      reads=[S_yn], writes=[ynin], partial=(q2 > 0))
            P.I("act", "activation", [ucin], [ucb], out=ucb[:], in_=ucin[:], func=AF.Square)
            b1 = P.bank()
            for k in range(8):
                P.mm(b1, b1[:], onesb, onesb[:], ucin, ucin[:, k, :], k == 0, k == 7)
            b2 = P.bank()
            for k in range(8):
                P.mm(b2, b2[:], onesb, onesb[:], ucb, ucb[:, k, :], k == 0, k == 7)
            P.I("dve", "tensor_scalar", [b1], [mean], out=mean[:], in0=b1[:], scalar1=1.0 / 1024.0, scalar2=None, op0=ALU.mult)
            P.I("dve", "tensor_tensor", [mean], [msq], out=msq[:], in0=mean[:], in1=mean[:], op=ALU.mult)
            P.I("dve", "scalar_tensor_tensor", [b2, msq], [Av], out=Av[:], in0=b2[:], scalar=1.0 / 1024.0, in1=msq[:],
                op0=ALU.mult, op1=ALU.subtract)
            P.I("act", "activation", [Av, epsb], [Av], out=Av[:], in_=Av[:], func=AF.Sqrt, bias=epsb[:])
            P.I("dve", "reciprocal", [Av], [Av], out=Av[:], in_=Av[:])
            P.I("dve", "scalar_tensor_tensor", [mean, Av], [Bv], out=Bv[:], in0=mean[:], scalar=-1.0, in1=Av[:],
                op0=ALU.mult, op1=ALU.mult)
            for k in range(8):
                t_ = tk[k % 2]
                P.I("dve", "tensor_tensor", [ucin, Av], [t_], out=t_[:], in0=ucin[:, k, :], in1=Av[:], op=ALU.mult)
                P.I("dve", "tensor_tensor", [t_, Bv], [t_], out=t_[:], in0=t_[:], in1=Bv[:], op=ALU.add)
                P.I("act", "activation", [t_, sp_["clg"], sp_["clb"]], [ucb], partial=(k > 0), out=ucb[:, k, :], in_=t_[:], func=AF.Silu,
                    scale=sp_["clg"][:, k:k + 1], bias=sp_["clb"][:, k:k + 1])

        def c_main_a(T):
            ts_ = slice(T * 512, (T + 1) * 512)
            for j in range(8):
                g_ = gin[j % 2]
                P.dma("sp", g_[:, 0, :], S_g[j, :, ts_], g_, reads=[S_g], writes=[g_])
                P.dma("sp", g_[:, 1, :], S_g[8 + j, :, ts_], g_, reads=[S_g], writes=[g_], partial=True)
                b1 = P.bank()
                for k in range(8):
                    P.mm(b1, b1[:], cwo, cwo[:, k, j * 128:(j + 1) * 128], ucb, ucb[:, k, :], k == 0, k == 7)
                b2 = P.bank()
                for k in range(16):
                    P.mm(b2, b2[:], swo, swo[:, k, j * 128:(j + 1) * 128], ynin, ynin[:, k, :], k == 0, k == 15)
                a_ = ta[j % 2]
                b_ = tb[j % 2]
                P.I("dve", "tensor_tensor", [b1, g_], [a_], out=a_[:], in0=b1[:], in1=g_[:, 0, :], op=ALU.mult)
                P.I("dve", "tensor_tensor", [b2, g_], [b_], out=b_[:], in0=b2[:], in1=g_[:, 1, :], op=ALU.mult)
                P.I("dve", "tensor_tensor", [a_, b_], [hT], partial=(j > 0), out=hT[:, j, :], in0=a_[:], in1=b_[:], op=ALU.add)

        def c_main_b(T):
            for sub in range(4):
                tok0 = T * 512 + sub * 128
                xr = xres[sub % 2]
                P.dma("sp", xr[:], x_src[tok0:tok0 + 128, :], xr, reads=[x_src], writes=[xr])
                for hf in range(2):
                    bk = P.bank()
                    for k in range(8):
                        P.mm(bk, bk[:], hT, hT[:, k, sub * 128:(sub + 1) * 128], wo, wo[:, k, hf * 512:(hf + 1) * 512], k == 0, k == 7)
                    P.I("dve", "scalar_tensor_tensor", [xr, bk], [vv4], partial=True, out=vv4[:, sub, hf * 512:(hf + 1) * 512],
                        in0=xr[:, hf * 512:(hf + 1) * 512], scalar=ALPHA, in1=bk[:], op0=ALU.mult, op1=ALU.add)
                for i_ in range(2):
                    P.I("dve", "bn_stats", [vv4], [st6], partial=(i_ > 0), out=st6[:, i_, :], in_=vv4[:, sub, i_ * 512:(i_ + 1) * 512])
                P.I("dve", "bn_aggr", [st6], [mv4], partial=True, out=mv4[:, sub, :], in_=st6[:])
            P.I("act", "activation", [mv4, epsb], [rs4c], out=rs4c[:], in_=mv4[:, :, 1], func=AF.Ln, bias=epsb[:])
            P.I("act", "activation", [rs4c], [rs4c], out=rs4c[:], in_=rs4c[:], func=AF.Exp, scale=-0.5)
            P.I("dve", "scalar_tensor_tensor", [mv4, rs4c], [nm4], out=nm4[:], in0=mv4[:, :, 0], scalar=-1.0, in1=rs4c[:],
                op0=ALU.mult, op1=ALU.mult)
            for sub in range(4):
                tok0 = T * 512 + sub * 128
                o = xn[sub % 2]
                P.I("act", "activation", [vv4, rs4c, nm4], [o], out=o[:], in_=vv4[:, sub, :], func=AF.Identity,
                    scale=rs4c[:, sub:sub + 1], bias=nm4[:, sub:sub + 1])
                P.I("dve", "tensor_tensor", [o, lng], [o], out=o[:], in0=o[:], in1=lng[:], op=ALU.mult)
                P.I("dve", "tensor_tensor", [o, lnb], [o], out=o[:], in0=o[:], in1=lnb[:], op=ALU.add)
                P.dma("sp", S_x1[tok0:tok0 + 128, :], o[:], o, reads=[o], writes=[S_x1], partial=True)

        c_prep(0)
        for T in range(4):
            c_main_a(T)
            if T + 1 < 4:
                c_prep(T + 1)
            c_main_b(T)
        P.release(mC)
        P.off = mBC
        if STOP == (l, "C"):
            P.barrier()
            P.emit()
            return P

        mD = P.mark()
        moe = (l % 2 == 1) and not FORCE_DENSE
        x1T = P.sb("x1T", [128, 8, L], BF16)
        P.dma("sp", lng[:], w["lfg"], lng, writes=[lng])
        P.dma("sp", lnb[:], w["lfb"], lnb, writes=[lnb])
        xin = [P.sb(f"x1in{i}", [128, 1024], F32) for i in range(2)]
        xinb = [P.sb(f"x1inb{i}", [128, 1024], BF16) for i in range(2)]
        for tt in range(NT):
            xtb = xinb[tt % 2]
            P.dma("pool", xtb[:], S_x1[tt * 128:(tt + 1) * 128, :], xtb, reads=[S_x1], writes=[xtb])
            bk = P.bank()
            bkv = bk.t.bitcast(BF16)
            for k in range(8):
                P.tr(bk, bkv[:, k * 128:(k + 1) * 128], xtb, xtb[:, k * 128:(k + 1) * 128], identb)
            P.I("act", "copy", [bk], [x1T], partial=True, out=x1T[:, :, tt * 128:(tt + 1) * 128],
                in_=bkv[:, :].rearrange("p (j t) -> p j t", t=128))
        if STOP == (l, "R"):
            P.barrier()
            P.emit()
            return P
        nexp = (NEXP_RUN if moe else 1)
        nfb = 28 if moe else 22
        FG = 7
        gsz = [7, 7, 7, 7] if moe else [6, 6, 5, 5]
        ngrp = 4
        wgu_d = wgu1 if moe else wgu0
        wdn_d = wd1 if moe else wd0
        wg = [P.sb(f"wgu{i}", [128, 8, 256], BF16) for i in range(3)]
        wdn = [P.sb(f"wdn{i}", [128, FG, 1024], BF16) for i in range(2)]
        hTt = [P.sb(f"hTt{i}", [128, FG, 1024], BF16) for i in range(2)]
        yacc = P.sb("yacc", [128, 8, 1024], F32)
        sgt = [P.sb(f"sgt{i}", [128, 512], F32) for i in range(2)]
        vv = P.sb("vv2", [128, 1024], F32)
        xo_ = [P.sb(f"xout{i}", [128, 1024], F32) for i in range(2)]
        st6 = P.sb("st6b", [128, 2, 6], F32)
        mv = P.sb("mvb", [128, 2], F32)
        rstd = P.sb("rstdb", [128, 1], F32)
        nmr = P.sb("nmrb", [128, 1], F32)
        mv8 = P.sb("mv8", [128, 8, 2], F32)
        rs8 = P.sb("rs8", [128, 8], F32)
        nm8 = P.sb("nm8", [128, 8], F32)
        pend_router = []
        if moe:
            lg = P.sb("lg", [128, 16, 8], F32)
            m8 = P.sb("m8", [128, 16, 8], F32)
            ex = P.sb("ex", [128, 16, 8], F32)
            nm1 = P.sb("nm1", [128, 16], F32)
            den = P.sb("den", [128, 16], F32)
            P.dma("sp", yacc[:], wr_d, yacc, writes=[yacc])
            P.I("dve", "memset", [], [lg], ap=lg[:], constant=0.0)

            def router_tile(tt):
                xt = xin[tt % 2]
                P.dma("sp", xt[:], S_x1[tt * 128:(tt + 1) * 128, :], xt, reads=[S_x1], writes=[xt])
                for e_ in range(NEXP):
                    P.I("dve", "scalar_tensor_tensor", [xt, yacc], [vv, lg], partial=True, out=vv[:], in0=xt[:], scalar=1.0,
                        in1=yacc[:, e_, :], op0=ALU.mult, op1=ALU.mult, accum_out=lg[:, tt, e_:e_ + 1])

            def router_math():
                for tt in range(NT):
                    P.I("dve", "max", [lg], [m8], partial=(tt > 0), out=m8[:, tt, :], in_=lg[:, tt, :])
                P.I("dve", "tensor_scalar", [m8], [nm1], out=nm1[:], in0=m8[:, :, 0], scalar1=-1.0, scalar2=None, op0=ALU.mult)
                for tt in range(NT):
                    P.I("act", "activation", [lg, nm1], [ex], partial=(tt > 0), out=ex[:, tt, :], in_=lg[:, tt, :], func=AF.Exp,
                        bias=nm1[:, tt:tt + 1])
                    P.I("dve", "tensor_scalar", [lg, m8], [cwt], partial=(tt > 0), out=cwt[:, tt, :], in0=lg[:, tt, :],
                        scalar1=m8[:, tt, 1:2], scalar2=None, op0=ALU.is_ge)
                P.I("dve", "tensor_tensor", [cwt, ex], [cwt], out=cwt[:], in0=cwt[:], in1=ex[:], op=ALU.mult)
                P.I("dve", "tensor_tensor", [m8], [den], out=den[:], in0=m8[:, :, 1], in1=m8[:, :, 0], op=ALU.subtract)
                P.I("act", "activation", [den], [den], out=den[:], in_=den[:], func=AF.Exp)
                P.I("dve", "tensor_scalar", [den], [den], out=den[:], in0=den[:], scalar1=1.0, scalar2=None, op0=ALU.add)
                P.I("dve", "reciprocal", [den], [den], out=den[:], in_=den[:])
                P.I("dve", "tensor_tensor", [cwt, den], [cwt], out=cwt[:], in0=cwt[:],
                    in1=den[:].rearrange("p (t o) -> p t o", o=1).broadcast_to([128, 16, 8]), op=ALU.mult)

            for tt in range(NT):
                pend_router.append((router_tile, (tt,)))
            pend_router.append((router_math, ()))

        def layer_norm_rows2(v, o):
            for i_ in range(2):
                P.I("dve", "bn_stats", [v], [st6], partial=(i_ > 0), out=st6[:, i_, :], in_=v[:, i_ * 512:(i_ + 1) * 512])
            P.I("dve", "bn_aggr", [st6], [mv], out=mv[:], in_=st6[:])
            P.I("act", "activation", [mv, epsb], [rstd], out=rstd[:], in_=mv[:, 1:2], func=AF.Sqrt, bias=epsb[:])
            P.I("dve", "reciprocal", [rstd], [rstd], out=rstd[:], in_=rstd[:])
            P.I("dve", "scalar_tensor_tensor", [mv, rstd], [nmr], out=nmr[:], in0=mv[:, 0:1], scalar=-1.0, in1=rstd[:],
                op0=ALU.mult, op1=ALU.mult)
            P.I("act", "activation", [v, rstd, nmr], [o], out=o[:], in_=v[:], func=AF.Identity, scale=rstd[:], bias=nmr[:])
            P.I("dve", "tensor_tensor", [o, lng], [o], out=o[:], in0=o[:], in1=lng[:], op=ALU.mult)
            P.I("dve", "tensor_tensor", [o, lnb], [o], out=o[:], in0=o[:], in1=lnb[:], op=ALU.add)

        wq = []
        for T2 in range(2):
            for e in range(nexp):
                for f in range(nfb):
                    wq.append((e, f))
        nload = [0]

        def issue_load():
            i = nload[0]
            if i >= len(wq):
                return
            e, f = wq[i]
            slot = wg[i % 3]
            P.dma("pool", flat(slot[:]), wgu_d[e, f].rearrange("p k n -> p (k n)"), slot, writes=[slot])
            nload[0] += 1

        issue_load()
        issue_load()
        wi = 0
        gi = 0
        pend_tail = []
        for T2 in range(2):
            first_acc = True
            for e in range(nexp):
                for gq in range(ngrp):
                    wdb = wdn[gi % 2]
                    hTb = hTt[gi % 2]
                    gi += 1
                    FGq = gsz[gq]
                    r0 = sum(gsz[:gq]) * 128
                    P.dma("pool", wdb[:, 0:FGq, :], wdn_d[e, r0:r0 + FGq * 128, :].rearrange("(f p) n -> p f n", p=128), wdb, writes=[wdb])
                    for fl in range(FGq):
                        issue_load()
                        slot = wg[wi % 3]
                        wi += 1
                        for _ in range(3):
                            if pend_router:
                                fn_, a_ = pend_router.pop(0)
                                fn_(*a_)
                        for _ in range(3):
                            if pend_tail:
                                fn_, a_ = pend_tail.pop(0)
                                fn_(*a_)
                        bgs = [P.bank(), P.bank()]
                        bus = [P.bank(), P.bank()]
                        for k in range(8):
                            for s5 in range(2):
                                c0 = T2 * 1024 + s5 * 512
                                P.mm(bgs[s5], bgs[s5][:], slot, slot[:, k, 0:128], x1T, x1T[:, k, c0:c0 + 512], k == 0, k == 7)
                        for k in range(8):
                            for s5 in range(2):
                                c0 = T2 * 1024 + s5 * 512
                                P.mm(bus[s5], bus[s5][:], slot, slot[:, k, 128:256], x1T, x1T[:, k, c0:c0 + 512], k == 0, k == 7)
                        for s5 in range(2):
                            sg_ = sgt[s5]
                            P.I("act", "activation", [bgs[s5]], [sg_], out=sg_[:], in_=bgs[s5][:], func=AF.Silu)
                            P.I("dve", "tensor_tensor", [bus[s5], sg_], [hTb], partial=True, out=hTb[:, fl, s5 * 512:(s5 + 1) * 512],
                                in0=bus[s5][:], in1=sg_[:], op=ALU.mult)
                    for sub in range(8):
                        bks = [P.bank(), P.bank()]
                        for fl in range(FGq):
                            for hf in range(2):
                                P.mm(bks[hf], bks[hf][:], hTb, hTb[:, fl, sub * 128:(sub + 1) * 128], wdb, wdb[:, fl, hf * 512:(hf + 1) * 512],
                                     fl == 0, fl == FGq - 1)
                        for hf in range(2):
                            bk = bks[hf]
                            ya = yacc[:, sub, hf * 512:(hf + 1) * 512]
                            tt = T2 * 8 + sub
                            if moe:
                                csc = cwt[:, tt, e:e + 1]
                                if first_acc:
                                    P.I("dve", "tensor_scalar", [bk, cwt], [yacc], partial=True, out=ya, in0=bk[:], scalar1=csc, scalar2=None,
                                        op0=ALU.mult)
                                else:
                                    P.I("dve", "scalar_tensor_tensor", [bk, cwt, yacc], [yacc], partial=True, out=ya, in0=bk[:], scalar=csc,
                                        in1=ya, op0=ALU.mult, op1=ALU.add)
                            else:
                                if first_acc:
                                    P.I("dve", "tensor_copy", [bk], [yacc], partial=True, out=ya, in_=bk[:])
                                else:
                                    P.I("dve", "tensor_tensor", [bk, yacc], [yacc], partial=True, out=ya, in0=bk[:], in1=ya, op=ALU.add)
                    first_acc = False
            steps = []

            def stA(sub, T2=T2):
                tok0 = T2 * 1024 + sub * 128
                xt = xin[sub % 2]
                P.dma("sp", xt[:], S_x1[tok0:tok0 + 128, :], xt, reads=[S_x1], writes=[xt])
                P.I("dve", "scalar_tensor_tensor", [xt, yacc], [yacc], partial=True, out=yacc[:, sub, :], in0=xt[:], scalar=ALPHA,
                    in1=yacc[:, sub, :], op0=ALU.mult, op1=ALU.add)
                for i_ in range(2):
                    P.I("dve", "bn_stats", [yacc], [st6], partial=(i_ > 0), out=st6[:, i_, :], in_=yacc[:, sub, i_ * 512:(i_ + 1) * 512])
                P.I("dve", "bn_aggr", [st6], [mv8], partial=True, out=mv8[:, sub, :], in_=st6[:])

            def stB():
                P.I("act", "activation", [mv8, epsb], [rs8], out=rs8[:], in_=mv8[:, :, 1], func=AF.Sqrt, bias=epsb[:])
                P.I("dve", "reciprocal", [rs8], [rs8], out=rs8[:], in_=rs8[:])
                P.I("dve", "scalar_tensor_tensor", [mv8, rs8], [nm8], out=nm8[:], in0=mv8[:, :, 0], scalar=-1.0, in1=rs8[:],
                    op0=ALU.mult, op1=ALU.mult)

            def stC(sub, T2=T2):
                tok0 = T2 * 1024 + sub * 128
                o = xo_[sub % 2]
                P.I("act", "activation", [yacc, rs8, nm8], [o], out=o[:], in_=yacc[:, sub, :], func=AF.Identity,
                    scale=rs8[:, sub:sub + 1], bias=nm8[:, sub:sub + 1])
                P.I("dve", "tensor_tensor", [o, lng], [o], out=o[:], in0=o[:], in1=lng[:], op=ALU.mult)
                P.I("dve", "tensor_tensor", [o, lnb], [o], out=o[:], in0=o[:], in1=lnb[:], op=ALU.add)
                P.dma("sp", x_dst[tok0:tok0 + 128, :], o[:], o, reads=[o], writes=[x_dst], partial=True)

            for sub in range(8):
                steps.append((stA, (sub,)))
            steps.append((stB, ()))
            for sub in range(8):
                steps.append((stC, (sub,)))
            if T2 == 0:
                pend_tail.extend(steps)
            else:
                for fn_, a_ in steps:
                    fn_(*a_)
        P.release(mD)
        x_src = x_dst

    P.barrier()
    P.emit()
    return P


def _prep(inputs):
    f32 = np.float32
    sh = {}
    sh["c_identf"] = np.eye(128, dtype=f32)
    t = np.arange(128)
    sh["c_U"] = (t[:, None] <= t[None, :]).astype(f32)
    sh["c_Ls"] = (t[:, None] > t[None, :]).astype(f32)
    sh["c_ones"] = np.ones((128, 128), f32)
    sh["c_idst"] = np.concatenate([np.eye(64, dtype=f32), np.eye(64, dtype=f32)], axis=0)

    def pk(wm):
        n = wm.shape[1]
        return np.ascontiguousarray(wm.reshape(8, 128, n).transpose(1, 0, 2))

    def rep(v):
        return np.ascontiguousarray(np.broadcast_to(np.asarray(v, f32)[None, :], (128, v.shape[0])))

    def pp(v, nb):
        return np.ascontiguousarray(np.asarray(v, f32).reshape(nb, 128).T)

    for l in range(2):
        win = np.asarray(inputs["mix_w_in"][l], f32)
        cv, cg, z, xbc, dt, gates = np.split(win, np.cumsum([1024, 1024, 2048, 3072, 32])[:5].tolist(), axis=1)
        cols = []
        for i in range(8):
            cols.append(cg[:, i * 128:(i + 1) * 128])
            cols.append(cv[:, i * 128:(i + 1) * 128])
        cols.append(xbc)
        cols.append(gates)
        wa = np.concatenate(cols, axis=1)
        sh[f"win_a{l}"] = np.ascontiguousarray(pk(wa).reshape(128, 8, 14, 512).transpose(2, 0, 1, 3))
        sh[f"win_z{l}"] = np.ascontiguousarray(pk(z).reshape(128, 8, 4, 512).transpose(2, 0, 1, 3))
        sh[f"win_dt{l}"] = pk(dt)
        sh[f"cdw{l}"] = np.ascontiguousarray(np.asarray(inputs["conv_dw_w"][l], f32).reshape(31, 8, 128).transpose(2, 1, 0))
        sh[f"cdb{l}"] = pp(inputs["conv_dw_b"][l], 8)
        sh[f"clg{l}"] = pp(inputs["conv_ln_g"][l], 8)
        sh[f"clb{l}"] = pp(inputs["conv_ln_b"][l], 8)
        sh[f"scw{l}"] = np.ascontiguousarray(np.asarray(inputs["ssm_conv_w"][l], f32).reshape(4, 24, 128).transpose(2, 1, 0))
        sh[f"scb{l}"] = pp(inputs["ssm_conv_b"][l], 24)
        sh[f"dtb{l}"] = rep(inputs["ssm_dt_bias"][l])
        sh[f"alog{l}"] = rep(inputs["ssm_a_log"][l])
        dd = np.asarray(inputs["ssm_d"][l], f32)
        dppv = np.zeros((128, 16), f32)
        dppv[:64, :] = dd[0::2][None, :]
        dppv[64:, :] = dd[1::2][None, :]
        sh[f"dpp{l}"] = dppv
        sh[f"nw{l}"] = pp(inputs["ssm_norm_w"][l], 16)
        sh[f"cwo{l}"] = pk(np.asarray(inputs["conv_w_out"][l], f32))
        sw = np.asarray(inputs["ssm_w_out"][l], f32)
        sh[f"swo{l}"] = np.ascontiguousarray(sw.reshape(16, 128, 1024).transpose(1, 0, 2))
        sh[f"wo{l}"] = pk(np.asarray(inputs["mix_w_out"][l], f32))
        sh[f"lmg{l}"] = rep(inputs["ln_mix_g"][l])
        sh[f"lmb{l}"] = rep(inputs["ln_mix_b"][l])
        sh[f"lfg{l}"] = rep(inputs["ln_ffn_g"][l])
        sh[f"lfb{l}"] = rep(inputs["ln_ffn_b"][l])

    def gu(wgm, wum, nfb):
        a = pk(wgm).reshape(128, 8, nfb, 128)
        b = pk(wum).reshape(128, 8, nfb, 128)
        return np.ascontiguousarray(np.concatenate([a, b], axis=3).transpose(2, 0, 1, 3))

    sh["wgu0"] = gu(np.asarray(inputs["ffn_w_gate"][0], f32), np.asarray(inputs["ffn_w_up"][0], f32), 22)[None]
    sh["wd0"] = np.ascontiguousarray(np.asarray(inputs["ffn_w_down"], f32))
    sh["wgu1"] = np.stack([gu(np.asarray(inputs["moe_w_gate"][0, e], f32), np.asarray(inputs["moe_w_up"][0, e], f32), 28)
                           for e in range(NEXP)], axis=0)
    sh["wd1"] = np.ascontiguousarray(np.asarray(inputs["moe_w_down"][0], f32))
    sh["wr"] = np.ascontiguousarray(np.broadcast_to(np.asarray(inputs["moe_router"][0], f32).T[None], (128, 8, 1024)))
    return sh


def kernel(**inputs):
    x = np.asarray(inputs["x"], np.float32)
    sh = _prep(inputs)
    nc = bass.Bass("TRN2", target_bir_lowering=False)
    build(nc)
    in_maps = []
    for c in range(8):
        m = dict(sh)
        m["x"] = np.ascontiguousarray(x[c])
        in_maps.append(m)
    res = run_bass_kernel_spmd(nc, in_maps, core_ids=list(range(8)))
    return np.stack([np.asarray(r["out"], np.float32) for r in res.results], axis=0)
```

```python
import numpy as np
import concourse.bass as bass
import concourse.mybir as mybir
from concourse.bass_utils import run_bass_kernel_spmd

F32 = mybir.dt.float32
BF16 = mybir.dt.bfloat16
U8 = mybir.dt.uint8
AF = mybir.ActivationFunctionType
ALU = mybir.AluOpType

L = 2048
D = 1024
NT = 16
H = 32
ALPHA = 4.0 ** 0.25
EPS = 1e-5
FF_DENSE = 2816
FF_EXP = 3584
NEXP = 8
NEXP_RUN = 8
STOP = None
FORCE_DENSE = False
ARENA = 204 * 1024


class Buf:
    def __init__(self, name, t):
        self.name = name
        self.t = t
        self.w = {}
        self.wf = {}
        self.r = {}
        self.dsem = None

    def __getitem__(self, k):
        return self.t[k]


class Prog:
    ENG = ("pe", "act", "dve", "pool", "sp")

    def __init__(self, nc):
        self.nc = nc
        self.ops = {e: [] for e in self.ENG}
        self.sem_names = []
        self.sem_cnt = []
        self.esem = {}
        for e in self.ENG:
            self.esem[e] = self.new_sem("e_" + e)
        self.sem_eng = {v: k for k, v in self.esem.items()}
        self.last_op = {}
        self.seen = {e: {} for e in self.ENG}
        self._ctx = []
        cm = nc.sbuf_tensor("arena", [128, ARENA], U8)
        self.arena = cm.__enter__()
        self._ctx.append(cm)
        self.off = 0
        self.banks = []
        for i in range(8):
            cm = nc.psum_tensor("bank%d" % i, [128, 512], F32)
            self.banks.append(Buf("bank%d" % i, cm.__enter__()))
            self._ctx.append(cm)
        self.bank_i = 0
        self.n_instr = 0
        self.dsem_by_name = {}

    def new_sem(self, name):
        name = "%s_%d" % (name, len(self.sem_names))
        self.sem_names.append(name)
        self.sem_cnt.append(0)
        return len(self.sem_names) - 1

    def bank(self):
        b = self.banks[self.bank_i % 8]
        self.bank_i += 1
        return b

    def sb(self, name, shape, dt):
        esz = 4 if dt == F32 else 2
        n = 1
        for s in shape[1:]:
            n *= s
        off = (self.off + 63) // 64 * 64
        assert off + n * esz <= ARENA, ("SBUF arena overflow", name, off, n * esz)
        ap = self.arena[:, off:off + n * esz].bitcast(dt)
        if len(shape) == 3:
            ap = ap.rearrange("p (a b) -> p a b", b=shape[2])
        self.off = off + n * esz
        self.hw = max(getattr(self, "hw", 0), self.off)
        return Buf(name, ap)

    def mark(self):
        return self.off

    def release(self, m):
        self.barrier()
        self.hwlog = getattr(self, "hwlog", []) + [self.hw]
        self.hw = 0
        self.off = m

    def _collect(self, eng, reads, writes, partial):
        need = {}
        for b in reads:
            for s, v in b.w.items():
                if need.get(s, 0) < v:
                    need[s] = v
        for b in writes:
            for s, v in (b.wf if partial else b.w).items():
                if need.get(s, 0) < v:
                    need[s] = v
            for s, v in b.r.items():
                if need.get(s, 0) < v:
                    need[s] = v
        waits = []
        seen = self.seen[eng]
        for s, v in need.items():
            if eng == "pe" and s == self.esem["pe"]:
                continue
            if seen.get(s, 0) >= v:
                continue
            self._materialize(s, v)
            seen[s] = v
            waits.append((s, v))
        return waits

    def _materialize(self, s, v):
        e = self.sem_eng.get(s)
        if e is None or v <= self.sem_cnt[s]:
            return
        assert v == self.sem_cnt[s] + 1
        ent = self.ops[e][self.last_op[e]]
        assert ent[2] is None
        ent[2] = (s, 1)
        self.sem_cnt[s] += 1

    def _commit(self, ev_s, ev_v, reads, writes, partial):
        for b in reads:
            if b.r.get(ev_s, 0) < ev_v:
                b.r[ev_s] = ev_v
        for b in writes:
            if partial:
                b.w[ev_s] = ev_v
            else:
                b.w = {ev_s: ev_v}
                b.wf = {ev_s: ev_v}
                b.r = {}

    def op(self, eng, fn, reads=(), writes=(), partial=False):
        waits = self._collect(eng, reads, writes, partial)
        s = self.esem[eng]
        self.ops[eng].append([waits, fn, None])
        self.last_op[eng] = len(self.ops[eng]) - 1
        self._commit(s, self.sem_cnt[s] + 1, reads, writes, partial)
        self.n_instr += 1

    def I(self, eng, name, reads, writes, partial=False, **kw):
        self.op(eng, lambda e, name=name, kw=kw: getattr(e, name)(**kw), reads, writes, partial)

    def mm(self, ob, out, lb, lhsT, rb, rhs, start, stop):
        self.op("pe", lambda e: e.matmul(out, lhsT, rhs, start=start, stop=stop), [lb, rb], [ob])

    def tr(self, ob, out, ib, in_, idb):
        self.op("pe", lambda e: e.transpose(out, in_, idb.t), [ib, idb], [ob])

    def dma(self, q, out_ap, in_ap, key, reads=(), writes=(), partial=False):
        if key.dsem is None:
            if key.name not in self.dsem_by_name:
                self.dsem_by_name[key.name] = self.new_sem("d_" + key.name)
            key.dsem = self.dsem_by_name[key.name]
        waits = self._collect(q, reads, writes, partial)
        s = key.dsem
        self.sem_cnt[s] += 16

        def fn(e):
            return e.dma_start(out=out_ap, in_=in_ap)
        self.ops[q].append([waits, fn, (s, 16)])
        self._commit(s, self.sem_cnt[s], reads, writes, partial)
        self.n_instr += 1

    def barrier(self):
        for e in self.ENG:
            li = self.last_op.get(e)
            if li is not None and self.ops[e][li][2] is None:
                self._materialize(self.esem[e], self.sem_cnt[self.esem[e]] + 1)
        allw = [(s, c) for s, c in enumerate(self.sem_cnt) if c > 0]
        for e in self.ENG:
            waits = []
            for s, v in allw:
                if self.seen[e].get(s, 0) >= v:
                    continue
                self.seen[e][s] = v
                waits.append((s, v))
            self.ops[e].append([waits, None, None])

    def emit(self):
        nc = self.nc
        sem_cms = [nc.semaphore(n) for n in self.sem_names]
        sems = [cm.__enter__() for cm in sem_cms]
        ops = self.ops

        def run(e, lst):
            for waits, fn, inc in lst:
                for s, v in waits:
                    e.wait_ge(sems[s], v)
                if fn is not None:
                    ins = fn(e)
                    if inc is not None:
                        ins.then_inc(sems[inc[0]], inc[1])

        with nc.Block() as block:
            @block.tensor
            def _(e):
                run(e, ops["pe"])

            @block.scalar
            def _(e):
                run(e, ops["act"])

            @block.vector
            def _(e):
                run(e, ops["dve"])

            @block.gpsimd
            def _(e):
                run(e, ops["pool"])

            @block.sync
            def _(e):
                run(e, ops["sp"])
        for cm in reversed(sem_cms):
            cm.__exit__(None, None, None)
        for cm in reversed(self._ctx):
            cm.__exit__(None, None, None)


def build(nc, nlayers=2, debug=False):
    P = Prog(nc)
    skind = "ExternalOutput" if debug else "Internal"

    def din(name, shape, dt=F32):
        return nc.dram_tensor(name, list(shape), dt, kind="ExternalInput").ap()

    def dscr(name, shape, dt):
        return P_dram(name, nc.dram_tensor(name, list(shape), dt, kind=skind).ap())

    def P_dram(name, ap):
        return Buf(name, ap)

    x_d = din("x", [L, D])
    out_d = nc.dram_tensor("out", [L, D], F32, kind="ExternalOutput").ap()
    c_identf = din("c_identf", [128, 128])
    c_U = din("c_U", [128, 128])
    c_Ls = din("c_Ls", [128, 128])
    c_ones = din("c_ones", [128, 128])
    c_idst = din("c_idst", [128, 64])
    Wd = []
    for l in range(2):
        w = {}
        w["win_a"] = din(f"win_a{l}", [14, 128, 8, 512])
        w["win_z"] = din(f"win_z{l}", [4, 128, 8, 512])
        w["win_dt"] = din(f"win_dt{l}", [128, 8, 32])
        w["cdw"] = din(f"cdw{l}", [128, 8, 31])
        w["cdb"] = din(f"cdb{l}", [128, 8])
        w["clg"] = din(f"clg{l}", [128, 8])
        w["clb"] = din(f"clb{l}", [128, 8])
        w["scw"] = din(f"scw{l}", [128, 24, 4])
        w["scb"] = din(f"scb{l}", [128, 24])
        w["dtb"] = din(f"dtb{l}", [128, 32])
        w["alog"] = din(f"alog{l}", [128, 32])
        w["dpp"] = din(f"dpp{l}", [128, 16])
        w["nw"] = din(f"nw{l}", [128, 16])
        w["cwo"] = din(f"cwo{l}", [128, 8, 1024])
        w["swo"] = din(f"swo{l}", [128, 16, 1024])
        w["wo"] = din(f"wo{l}", [128, 8, 1024])
        w["lmg"] = din(f"lmg{l}", [128, 1024])
        w["lmb"] = din(f"lmb{l}", [128, 1024])
        w["lfg"] = din(f"lfg{l}", [128, 1024])
        w["lfb"] = din(f"lfb{l}", [128, 1024])
        Wd.append(w)
    wgu0 = din("wgu0", [1, 22, 128, 8, 256])
    wd0 = din("wd0", [1, FF_DENSE, 1024])
    wgu1 = din("wgu1", [NEXP, 28, 128, 8, 256])
    wd1 = din("wd1", [NEXP, FF_EXP, 1024])
    wr_d = din("wr", [128, 8, 1024])

    identf = P.sb("identf", [128, 128], F32)
    identb = P.sb("identb", [128, 128], BF16)
    Um = P.sb("Um", [128, 128], F32)
    Ls = P.sb("Ls", [128, 128], F32)
    onesf = P.sb("onesf", [128, 128], F32)
    onesb = P.sb("onesb", [128, 128], BF16)
    idst = P.sb("idst", [128, 64], BF16)
    for b, d in ((identf, c_identf), (Um, c_U), (Ls, c_Ls), (onesf, c_ones)):
        P.dma("sp", b[:], d, b, writes=[b])
    P.dma("pool", identb[:], c_identf, identb, writes=[identb])
    P.dma("pool", onesb[:], c_ones, onesb, writes=[onesb])
    P.dma("pool", idst[:], c_idst, idst, writes=[idst])

    sp_ = {}
    for nm, shp in (("cdw", [128, 8, 31]), ("cdb", [128, 8]), ("clg", [128, 8]), ("clb", [128, 8]),
                    ("scw", [128, 24, 4]), ("scb", [128, 24]), ("dtb", [128, 32]), ("alog", [128, 32]),
                    ("dpp", [128, 16]), ("nw", [128, 16])):
        sp_[nm] = P.sb(nm, shp, F32)
    aneg = P.sb("aneg", [128, 32], F32)
    diagD = P.sb("diagD", [128, 16, 64], BF16)
    dtv = P.sb("dtv", [128, 16, 32], F32)
    dav = P.sb("dav", [128, 16, 32], F32)
    eA = P.sb("eA", [128, 16, 32], F32)
    eR = P.sb("eR", [128, 16, 32], F32)
    eT = P.sb("eT", [128, 16, 32], F32)
    dtR = P.sb("dtR", [128, 16, 32], F32)
    cwt = P.sb("cwt", [128, 16, 8], F32)
    lng = P.sb("lng", [128, 1024], F32)
    lnb = P.sb("lnb", [128, 1024], F32)
    epsb = P.sb("epsb", [128, 1], F32)
    P.I("dve", "memset", [], [epsb], ap=epsb[:], constant=EPS)

    def flat(ap3):
        return ap3.rearrange("p a b -> p (a b)")

    x_src = P_dram("x_in", x_d)

    for l in range(nlayers):
        w = Wd[l]
        last = (l == nlayers - 1)
        S_u = dscr(f"S_u{l}", [8, 128, L], BF16)
        S_xbc = dscr(f"S_xbc{l}", [24, 128, L], BF16)
        S_z = dscr(f"S_z{l}", [L, 2048], BF16)
        S_g = dscr(f"S_g{l}", [16, 128, L], BF16)
        S_yn = dscr(f"S_yn{l}", [16, 128, L], BF16)
        S_x1 = dscr(f"S_x1{l}", [L, D], F32)
        if last and nlayers == 2:
            x_dst = P_dram("out", out_d)
        elif last:
            x_dst = P_dram("out", out_d)
        else:
            x_dst = dscr(f"S_xl{l}", [L, D], F32)

        for nm in ("cdw", "cdb", "clg", "clb", "scw", "scb", "dtb", "alog", "dpp", "nw"):
            b = sp_[nm]
            P.dma("sp", b[:], w[nm], b, writes=[b])
        P.I("act", "activation", [sp_["alog"]], [aneg], out=aneg[:], in_=sp_["alog"][:], func=AF.Exp)
        P.I("dve", "tensor_scalar", [aneg], [aneg], out=aneg[:], in0=aneg[:], scalar1=-1.0, scalar2=None, op0=ALU.mult)
        for b_ in range(16):
            P.I("pool", "tensor_scalar", [idst, sp_["dpp"]], [diagD], partial=True, out=diagD[:, b_, :], in0=idst[:],
                scalar1=sp_["dpp"][:, b_:b_ + 1], scalar2=None, op0=ALU.mult)

        m_layer = P.mark()
        xT = P.sb("xT", [128, 8, L], BF16)

        mX = P.mark()
        xin = [P.sb(f"xin{i}", [128, 1024], BF16) for i in range(3)]
        for tt in range(NT):
            xt = xin[tt % 3]
            P.dma("pool", xt[:], x_src[tt * 128:(tt + 1) * 128, :], xt, reads=[x_src], writes=[xt])
            bk = P.bank()
            bkv = bk.t.bitcast(BF16)
            for k in range(8):
                P.tr(bk, bkv[:, k * 128:(k + 1) * 128], xt, xt[:, k * 128:(k + 1) * 128], identb)
            P.I("act", "copy", [bk], [xT], partial=True, out=xT[:, :, tt * 128:(tt + 1) * 128],
                in_=bkv[:, :].rearrange("p (j t) -> p j t", t=128))
        P.release(mX)

        mA = P.mark()
        wsl = [P.sb(f"wsl{i}", [128, 8, 512], BF16) for i in range(3)]
        sg = [P.sb(f"sg{i}", [128, L], BF16) for i in range(2)]
        upad = [P.sb(f"upad{i}", [128, 30 + L], BF16) for i in range(2)]
        dgw = [P.sb(f"dgw{i}", [128, 31, 128], BF16) for i in range(2)]
        uco = [P.sb(f"uco{i}", [128, L], BF16) for i in range(2)]
        xp = [P.sb(f"xp{i}", [128, 4 + L], F32) for i in range(2)]
        acc = [P.sb(f"acc{i}", [128, L], F32) for i in range(2)]
        xo = [P.sb(f"xo{i}", [128, L], BF16) for i in range(2)]
        go = [P.sb(f"go{i}", [128, L], BF16) for i in range(2)]
        for i in range(2):
            P.I("dve", "memset", [], [upad[i]], ap=upad[i][:, 0:30], constant=0.0)
            P.I("dve", "memset", [], [xp[i]], ap=xp[i][:, 0:3], constant=0.0)

        def load_slab(src, s, slot):
            P.dma("pool", flat(wsl[slot][:]), src[s].rearrange("p k n -> p (k n)"), wsl[slot], writes=[wsl[slot]])

        def conv31(i):
            up = upad[i % 2]
            dg = dgw[i % 2]
            uo = uco[i % 2]
            for t in range(4):
                bk = P.bank()
                for k in range(31):
                    P.mm(bk, bk[:], dg, dg[:, k, :], up, up[:, t * 512 + k:t * 512 + k + 512], k == 0, k == 30)
                P.I("act", "activation", [bk, sp_["cdb"]], [uo], partial=True, out=uo[:, t * 512:(t + 1) * 512], in_=bk[:],
                    func=AF.Identity, bias=sp_["cdb"][:, i:i + 1])
            P.dma("sp", S_u[i], uo[:], uo, reads=[uo], writes=[S_u], partial=True)

        load_slab(w["win_a"], 0, 0)
        load_slab(w["win_a"], 1, 1)
        pend_conv = None
        pend_silu = []
        for s in range(14):
            if s + 2 < 14:
                load_slab(w["win_a"], s + 2, (s + 2) % 3)
            ws = wsl[s % 3]
            for j in range(4):
                gb = s * 4 + j
                if gb < 16:
                    kind, idx = ("cg", gb // 2) if gb % 2 == 0 else ("cv", gb // 2)
                elif gb < 40:
                    kind, idx = "xbc", gb - 16
                else:
                    kind, idx = "gate", gb - 40
                if kind == "cg":
                    dg = dgw[idx % 2]
                    P.I("dve", "tensor_tensor", [identb, sp_["cdw"]], [dg], out=dg[:],
                        in0=identb[:].rearrange("p (o c) -> p o c", o=1).broadcast_to([128, 31, 128]),
                        in1=sp_["cdw"][:, idx, :].rearrange("p (k o) -> p k o", o=1).broadcast_to([128, 31, 128]), op=ALU.mult)
                for hf in range(2):
                    bks = [P.bank(), P.bank()]
                    for k in range(8):
                        for t in range(2):
                            c0 = hf * 1024 + t * 512
                            P.mm(bks[t], bks[t][:], ws, ws[:, k, j * 128:(j + 1) * 128], xT, xT[:, k, c0:c0 + 512], k == 0, k == 7)
                    for t in range(2):
                        c0 = hf * 1024 + t * 512
                        bk = bks[t]
                        if kind == "cg":
                            o = sg[idx % 2]
                            P.I("act", "activation", [bk], [o], partial=True, out=o[:, c0:c0 + 512], in_=bk[:], func=AF.Sigmoid)
                        elif kind == "cv":
                            o = upad[idx % 2]
                            P.I("dve", "tensor_tensor", [bk, sg[idx % 2]], [o], partial=True, out=o[:, 30 + c0:30 + c0 + 512],
                                in0=bk[:], in1=sg[idx % 2][:, c0:c0 + 512], op=ALU.mult)
                        elif kind == "xbc":
                            o = xp[idx % 2]
                            P.I("act", "copy", [bk], [o], partial=True, out=o[:, 3 + c0:3 + c0 + 512], in_=bk[:])
                        else:
                            o = go[idx % 2]
                            P.I("act", "activation", [bk], [o], partial=True, out=o[:, c0:c0 + 512], in_=bk[:], func=AF.Sigmoid)
                if kind == "cv":
                    if pend_conv is not None:
                        conv31(pend_conv)
                    pend_conv = idx
                elif kind == "xbc":
                    if pend_conv is not None:
                        conv31(pend_conv)
                        pend_conv = None
                    xpb = xp[idx % 2]
                    ac = acc[idx % 2]
                    P.I("act", "activation", [xpb, sp_["scw"], sp_["scb"]], [ac], out=ac[:], in_=xpb[:, 0:L], func=AF.Identity,
                        scale=sp_["scw"][:, idx, 0:1], bias=sp_["scb"][:, idx:idx + 1])
                    while pend_silu:
                        pend_silu.pop(0)()
                    for k in range(1, 4):
                        P.I("dve", "scalar_tensor_tensor", [xpb, sp_["scw"], ac], [ac], out=ac[:], in0=xpb[:, k:k + L],
                            scalar=sp_["scw"][:, idx, k:k + 1], in1=ac[:], op0=ALU.mult, op1=ALU.add)
                    def silu_out(idx=idx, ac=ac):
                        o = xo[idx % 2]
                        P.I("act", "activation", [ac], [o], out=o[:], in_=ac[:], func=AF.Silu)
                        P.dma("sp", S_xbc[idx], o[:], o, reads=[o], writes=[S_xbc], partial=True)
                    pend_silu.append(silu_out)
                elif kind == "gate":
                    while pend_silu:
                        pend_silu.pop(0)()
                    o = go[idx % 2]
                    P.dma("sp", S_g[idx], o[:], o, reads=[o], writes=[S_g], partial=True)
        P.release(mA)

        mA2 = P.mark()
        wsl = [P.sb(f"wslz{i}", [128, 8, 512], BF16) for i in range(3)]
        wdt = P.sb("wdt", [128, 8, 32], BF16)
        zo = [P.sb(f"zo{i}", [128, 512], BF16) for i in range(3)]
        P.dma("pool", wdt[:], w["win_dt"], wdt, writes=[wdt])
        load_slab(w["win_z"], 0, 0)
        load_slab(w["win_z"], 1, 1)
        for tt in range(NT):
            bk = P.bank()
            for k in range(8):
                P.mm(bk, bk[:, 0:32], xT, xT[:, k, tt * 128:(tt + 1) * 128], wdt, wdt[:, k, :], k == 0, k == 7)
            P.I("dve", "tensor_tensor", [bk, sp_["dtb"]], [dtv], partial=True, out=dtv[:, tt, :], in0=bk[:, 0:32],
                in1=sp_["dtb"][:], op=ALU.add)
        tmpa = P.sb("tmpa", [128, 512], F32)
        tmpb = P.sb("tmpb", [128, 512], F32)
        dtf = flat(dtv[:])
        P.I("dve", "tensor_scalar", [dtv], [tmpb], out=tmpb[:], in0=dtf, scalar1=0.0, scalar2=None, op0=ALU.max)
        P.I("dve", "tensor_scalar", [dtv], [tmpa], out=tmpa[:], in0=dtf, scalar1=0.0, scalar2=None, op0=ALU.min)
        P.I("dve", "tensor_tensor", [tmpa, tmpb], [tmpa], out=tmpa[:], in0=tmpa[:], in1=tmpb[:], op=ALU.subtract)
        P.I("act", "activation", [tmpa], [tmpa], out=tmpa[:], in_=tmpa[:], func=AF.Exp)
        P.I("act", "activation", [tmpa], [tmpa], out=tmpa[:], in_=tmpa[:], func=AF.Ln, bias=1.0)
        P.I("dve", "tensor_tensor", [tmpa, tmpb], [dtv], out=dtf, in0=tmpa[:], in1=tmpb[:], op=ALU.add)
        P.I("dve", "tensor_tensor", [dtv, aneg], [dav], out=dav[:], in0=dtv[:],
            in1=aneg[:].rearrange("p (o h) -> p o h", o=1).broadcast_to([128, 16, 32]), op=ALU.mult)
        for (lhs, dst) in ((Um, eA), (Ls, eR), (onesf, eT)):
            bk = P.bank()
            P.mm(bk, bk[:], lhs, lhs[:], dav, flat(dav[:]), True, True)
            P.I("act", "activation", [bk], [dst], out=flat(dst[:]), in_=bk[:], func=AF.Exp)
        P.I("dve", "tensor_tensor", [dtv, eR], [dtR], out=dtR[:], in0=dtv[:], in1=eR[:], op=ALU.mult)
        for s in range(4):
            if s + 2 < 4:
                load_slab(w["win_z"], s + 2, (s + 2) % 3)
            ws = wsl[s % 3]
            for tt in range(NT):
                bk = P.bank()
                for k in range(8):
                    P.mm(bk, bk[:], xT, xT[:, k, tt * 128:(tt + 1) * 128], ws, ws[:, k, :], k == 0, k == 7)
                o = zo[(s * NT + tt) % 3]
                P.I("act", "activation", [bk], [o], out=o[:], in_=bk[:], func=AF.Silu)
                P.dma("sp", S_z[tt * 128:(tt + 1) * 128, s * 512:(s + 1) * 512], o[:], o, reads=[o], writes=[S_z], partial=True)
        P.release(mA2)
        P.release(m_layer)
        if STOP == (l, "A2"):
            P.barrier()
            P.emit()
            return P

        mB = P.mark()
        xb2 = [P.sb(f"xb{i}", [128, 24, 256], BF16) for i in range(2)]
        zt = [P.sb(f"zt{i}", [128, 2048], BF16) for i in range(2)]
        xdt2 = [P.sb(f"xdt{i}", [128, 2048], BF16) for i in range(2)]
        xdec2 = [P.sb(f"xdec{i}", [128, 2048], BF16) for i in range(2)]
        btok2 = [P.sb(f"btok{i}", [128, 512], BF16) for i in range(2)]
        MT2 = [P.sb(f"MT{i}", [128, 32, 128], BF16) for i in range(2)]
        rsg = P.sb("rsg", [128, 32, 128], F32)
        Eb = P.sb("Eb", [128, 32, 128], BF16)
        cbm = P.sb("cbm", [128, 4, 128], BF16)
        St = P.sb("St", [128, 2048], F32)
        Sbf = P.sb("Sbf", [128, 2048], BF16)
        t1 = [P.sb(f"t1{i}", [128, 512], F32) for i in range(2)]
        yv = [P.sb(f"yv{i}", [128, 512], F32) for i in range(2)]
        hg = P.sb("hg", [128, 2048], F32)
        junk = P.sb("junk", [128, 512], BF16)
        ss = P.sb("ss", [128, 4], F32)
        rs4 = P.sb("rs4", [128, 4], F32)
        hn = P.sb("hn", [128, 2048], BF16)
        yno = [P.sb(f"yno{i}", [128, 16, 128], BF16) for i in range(2)]
        P.I("dve", "memset", [], [St], ap=St[:], constant=0.0)
        P.I("dve", "memset", [], [Sbf], ap=Sbf[:], constant=0.0)
        bT0, bTh, bT2, bSeg0, bSeg1, bY, bYo, bSn = P.banks

        def bfv(bk):
            return bk.t.bitcast(BF16)

        def ssd_front(c):
            cc = c % 2
            xb = xb2[(c // 2) % 2]
            xdt, xdec, btok, MT = xdt2[c % 2], xdec2[c % 2], btok2[c % 2], MT2[c % 2]
            def load_pair(pr):
                xbp = xb2[pr % 2]
                for q4 in range(4):
                    P.dma("sp", xbp[:, q4 * 6:(q4 + 1) * 6, :],
                          S_xbc[q4 * 6:(q4 + 1) * 6, :, pr * 256:pr * 256 + 256].rearrange("b p t -> p b t"),
                          xbp, reads=[S_xbc], writes=[xbp], partial=(q4 > 0))
            if c == 0:
                load_pair(0)
            if cc == 1 and c + 1 < NT:
                load_pair((c + 1) // 2)
            z_ = zt[c % 2]
            P.dma("sp", z_[:], S_z[c * 128:(c + 1) * 128, :], z_, reads=[S_z], writes=[z_])
            cs = slice(cc * 128, (cc + 1) * 128)
            P.I("dve", "tensor_tensor", [Um, dav], [rsg], out=rsg[:],
                in0=Um[:].rearrange("p (o l) -> p o l", o=1).broadcast_to([128, 32, 128]),
                in1=dav[:, c, :].rearrange("p (h o) -> p h o", o=1).broadcast_to([128, 32, 128]), op=ALU.mult)
            for g in range(4):
                P.tr(bT2, bfv(bT2)[:, g * 128:(g + 1) * 128], xb, xb[:, 16 + g, cs], identb)
            P.I("act", "copy", [bT2], [btok], out=btok[:], in_=bfv(bT2)[:, 0:512])
            for g in range(4):
                P.mm(bT2, bT2[:, g * 128:(g + 1) * 128], xb, xb[:, 16 + g, cs], xb, xb[:, 20 + g, cs], True, True)
            P.I("dve", "tensor_tensor", [bT2, Um], [cbm], out=cbm[:], in0=bT2[:].rearrange("p (g l) -> p g l", l=128),
                in1=Um[:].rearrange("p (o l) -> p o l", o=1).broadcast_to([128, 4, 128]), op=ALU.mult)
            for i_ in range(2):
                for b_ in range(8):
                    P.tr(bT0, bfv(bT0)[:, b_ * 128:(b_ + 1) * 128], xb, xb[:, i_ * 8 + b_, cs], identb)
                hs = slice(i_ * 16, (i_ + 1) * 16)
                for (dst, sc) in ((xdt, dtv), (xdec, dtR)):
                    P.I("dve", "tensor_tensor", [bT0, sc], [dst], partial=(i_ > 0),
                        out=dst[:, i_ * 1024:(i_ + 1) * 1024].rearrange("p (h q) -> p h q", q=64),
                        in0=bfv(bT0)[:, :].rearrange("p (h q) -> p h q", q=64),
                        in1=sc[:, c, hs].rearrange("p (h o) -> p h o", o=1).broadcast_to([128, 16, 64]), op=ALU.mult)

        def ssd_seg(c, q):
            MT = MT2[c % 2]
            bk = bSeg0 if q % 2 == 0 else bSeg1
            P.mm(bk, bk[:], Ls, Ls[:], rsg, flat(rsg[:])[:, q * 512:(q + 1) * 512], True, True)
            P.I("act", "activation", [bk], [Eb], partial=(q > 0), out=flat(Eb[:])[:, q * 512:(q + 1) * 512], in_=bk[:], func=AF.Exp)
            if q % 2 == 1:
                g = q // 2
                P.I("dve", "tensor_tensor", [Eb, cbm], [MT], partial=(g > 0), out=MT[:, g * 8:(g + 1) * 8, :], in0=Eb[:, g * 8:(g + 1) * 8, :],
                    in1=cbm[:, g:g + 1, :].broadcast_to([128, 8, 128]), op=ALU.mult)

        def ssd_back(c, nxt):
            cc = c % 2
            xb = xb2[(c // 2) % 2]
            xdt, xdec, btok, MT = xdt2[c % 2], xdec2[c % 2], btok2[c % 2], MT2[c % 2]
            z_ = zt[c % 2]
            cs = slice(cc * 128, (cc + 1) * 128)
            P.I("dve", "memset", [], [ss], ap=ss[:], constant=0.0)
            for g in range(4):
                for r in range(8):
                    h = g * 8 + r
                    blk = h // 2
                    po = (h % 2) * 64
                    P.mm(bY, bY[:, r * 64:(r + 1) * 64], xb, xb[po:po + 64, blk, cs], diagD, diagD[po:po + 64, blk, :], True, False)
                    P.mm(bY, bY[:, r * 64:(r + 1) * 64], MT, MT[:, h, :], xdt, xdt[:, h * 64:(h + 1) * 64], False, True)
                if c > 0:
                    P.mm(bYo, bYo[:], xb, xb[:, 20 + g, cs], Sbf, Sbf[:, g * 512:(g + 1) * 512], True, True)
                if c < NT - 1:
                    P.mm(bSn, bSn[:], btok, btok[:, g * 128:(g + 1) * 128], xdec, xdec[:, g * 512:(g + 1) * 512], True, True)
                y_ = yv[g % 2]
                if c > 0:
                    t_ = t1[g % 2]
                    P.I("dve", "tensor_tensor", [bYo, eA], [t_], out=t_[:].rearrange("p (r q) -> p r q", q=64),
                        in0=bYo[:].rearrange("p (r q) -> p r q", q=64),
                        in1=eA[:, c, g * 8:(g + 1) * 8].rearrange("p (r o) -> p r o", o=1).broadcast_to([128, 8, 64]), op=ALU.mult)
                    P.I("dve", "tensor_tensor", [bY, t_], [y_], out=y_[:], in0=bY[:], in1=t_[:], op=ALU.add)
                else:
                    P.I("act", "copy", [bY], [y_], out=y_[:], in_=bY[:])
                P.I("dve", "tensor_tensor", [y_, z_], [hg], partial=(g > 0), out=hg[:, g * 512:(g + 1) * 512], in0=y_[:],
                    in1=z_[:, g * 512:(g + 1) * 512], op=ALU.mult)
                P.I("act", "activation", [hg], [junk, ss], out=junk[:], in_=hg[:, g * 512:(g + 1) * 512], func=AF.Square,
                    accum_out=ss[:, g:g + 1])
                if c < NT - 1:
                    sl = slice(g * 512, (g + 1) * 512)
                    P.I("dve", "tensor_tensor", [St, eT], [St], out=St[:, sl].rearrange("p (r q) -> p r q", q=64),
                        in0=St[:, sl].rearrange("p (r q) -> p r q", q=64),
                        in1=eT[:, c, g * 8:(g + 1) * 8].rearrange("p (r o) -> p r o", o=1).broadcast_to([128, 8, 64]), op=ALU.mult)
                    P.I("dve", "tensor_tensor", [St, bSn], [St], out=St[:, sl], in0=St[:, sl], in1=bSn[:], op=ALU.add)
                    P.I("act", "copy", [St], [Sbf], out=Sbf[:, sl], in_=St[:, sl])
                if nxt is not None:
                    ssd_seg(nxt, 2 * g)
                    ssd_seg(nxt, 2 * g + 1)
            P.I("act", "activation", [ss, epsb], [rs4], out=rs4[:], in_=ss[:], func=AF.Ln, scale=1.0 / 512.0, bias=epsb[:])
            P.I("act", "activation", [rs4], [rs4], out=rs4[:], in_=rs4[:], func=AF.Exp, scale=-0.5)
            for g in range(4):
                P.I("act", "activation", [hg, rs4], [hn], partial=(g > 0), out=hn[:, g * 512:(g + 1) * 512], in_=hg[:, g * 512:(g + 1) * 512],
                    func=AF.Identity, scale=rs4[:, g:g + 1])

        def ssd_tail(c):
            yo = yno[c % 2]
            for i_ in range(2):
                for b_ in range(8):
                    P.tr(bTh, bfv(bTh)[:, b_ * 128:(b_ + 1) * 128], hn, hn[:, (i_ * 8 + b_) * 128:(i_ * 8 + b_ + 1) * 128], identb)
                P.I("act", "copy", [bTh], [yo], partial=(i_ > 0), out=yo[:, i_ * 8:(i_ + 1) * 8, :],
                    in_=bfv(bTh)[:, :].rearrange("p (b t) -> p b t", t=128))
            P.dma("sp", S_yn[:, :, c * 128:(c + 1) * 128].rearrange("b p t -> p b t"), yo[:], yo, reads=[yo], writes=[S_yn], partial=True)

        ssd_front(0)
        for q in range(8):
            ssd_seg(0, q)
        for c in range(NT):
            if c + 1 < NT:
                ssd_front(c + 1)
            if c > 0:
                ssd_tail(c - 1)
            ssd_back(c, c + 1 if c + 1 < NT else None)
        ssd_tail(NT - 1)
        P.release(mB)
        if STOP == (l, "B"):
            P.barrier()
            P.emit()
            return P

        mC = P.mark()
        swo = P.sb("swo", [128, 16, 1024], BF16)
        cwo = P.sb("cwo", [128, 8, 1024], BF16)
        wo = P.sb("wo", [128, 8, 1024], BF16)
        P.dma("pool", flat(swo[:]), w["swo"].rearrange("p k n -> p (k n)"), swo, writes=[swo])
        P.dma("pool", flat(cwo[:]), w["cwo"].rearrange("p k n -> p (k n)"), cwo, writes=[cwo])
        P.dma("pool", flat(wo[:]), w["wo"].rearrange("p k n -> p (k n)"), wo, writes=[wo])
        for q2 in range(2):
            P.I("dve", "tensor_tensor", [swo, sp_["nw"]], [swo], partial=(q2 > 0), out=swo[:, q2 * 8:(q2 + 1) * 8, :], in0=swo[:, q2 * 8:(q2 + 1) * 8, :],
                in1=sp_["nw"][:, q2 * 8:(q2 + 1) * 8].rearrange("p (k o) -> p k o", o=1).broadcast_to([128, 8, 1024]), op=ALU.mult)
        P.dma("sp", lng[:], w["lmg"], lng, writes=[lng])
        P.dma("sp", lnb[:], w["lmb"], lnb, writes=[lnb])
        ucin = P.sb("ucin", [128, 8, 512], BF16)
        ucb = P.sb("ucb", [128, 8, 512], BF16)
        ynin = P.sb("ynin", [128, 16, 512], BF16)
        gin = [P.sb(f"gin{i}", [128, 2, 512], BF16) for i in range(2)]
        hT = P.sb("hT", [128, 8, 512], BF16)
        tk = [P.sb(f"tk{i}", [128, 512], F32) for i in range(2)]
        ta = [P.sb(f"ta{i}", [128, 512], F32) for i in range(2)]
        tb = [P.sb(f"tb{i}", [128, 512], F32) for i in range(2)]
        mean = P.sb("mean", [128, 512], F32)
        msq = P.sb("msq", [128, 512], F32)
        Av = P.sb("Av", [128, 512], F32)
        Bv = P.sb("Bv", [128, 512], F32)
        xres = [P.sb(f"xres{i}", [128, 1024], F32) for i in range(2)]
        vv4 = P.sb("vv4", [128, 4, 1024], F32)
        mv4 = P.sb("mv4", [128, 4, 2], F32)
        rs4c = P.sb("rs4c", [128, 4], F32)
        nm4 = P.sb("nm4", [128, 4], F32)
        xn = [P.sb(f"xn{i}", [128, 1024], F32) for i in range(2)]
        st6 = P.sb("st6", [128, 2, 6], F32)
        mv = P.sb("mv", [128, 2], F32)
        rstd = P.sb("rstd", [128, 1], F32)
        nmr = P.sb("nmr", [128, 1], F32)

        def layer_norm_rows(v, o, stq):
            for i_ in range(2):
                P.I("dve", "bn_stats", [v], [stq], partial=(i_ > 0), out=stq[:, i_, :], in_=v[:, i_ * 512:(i_ + 1) * 512])
            P.I("dve", "bn_aggr", [stq], [mv], out=mv[:], in_=stq[:])
            P.I("act", "activation", [mv, epsb], [rstd], out=rstd[:], in_=mv[:, 1:2], func=AF.Sqrt, bias=epsb[:])
            P.I("dve", "reciprocal", [rstd], [rstd], out=rstd[:], in_=rstd[:])
            P.I("dve", "scalar_tensor_tensor", [mv, rstd], [nmr], out=nmr[:], in0=mv[:, 0:1], scalar=-1.0, in1=rstd[:],
                op0=ALU.mult, op1=ALU.mult)
            P.I("act", "activation", [v, rstd, nmr], [o], out=o[:], in_=v[:], func=AF.Identity, scale=rstd[:], bias=nmr[:])
            P.I("dve", "tensor_tensor", [o, lng], [o], out=o[:], in0=o[:], in1=lng[:], op=ALU.mult)
            P.I("dve", "tensor_tensor", [o, lnb], [o], out=o[:], in0=o[:], in1=lnb[:], op=ALU.add)

        def c_prep(T):
            ts_ = slice(T * 512, (T + 1) * 512)
            P.dma("sp", ucin[:], S_u[:, :, ts_].rearrange("b p t -> p b t"), ucin, reads=[S_u], writes=[ucin])
            for q2 in range(2):
                P.dma("sp", ynin[:, q2 * 8:(q2 + 1) * 8, :], S_yn[q2 * 8:(q2 + 1) * 8, :, ts_].rearrange("b p t -> p b t"), ynin,
                      reads=[S_yn], writes=[ynin], partial=(q2 > 0))
            P.I("act", "activation", [ucin], [ucb], out=ucb[:], in_=ucin[:], func=AF.Square)
            b1 = P.bank()
            for k in range(8):
                P.mm(b1, b1[:], onesb, onesb[:], ucin, ucin[:, k, :], k == 0, k == 7)
            b2 = P.bank()
            for k in range(8):
                P.mm(b2, b2[:], onesb, onesb[:], ucb, ucb[:, k, :], k == 0, k == 7)
            P.I("dve", "tensor_scalar", [b1], [mean], out=mean[:], in0=b1[:], scalar1=1.0 / 1024.0, scalar2=None, op0=ALU.mult)
            P.I("dve", "tensor_tensor", [mean], [msq], out=msq[:], in0=mean[:], in1=mean[:], op=ALU.mult)
            P.I("dve", "scalar_tensor_tensor", [b2, msq], [Av], out=Av[:], in0=b2[:], scalar=1.0 / 1024.0, in1=msq[:],
                op0=ALU.mult, op1=ALU.subtract)
            P.I("act", "activation", [Av, epsb], [Av], out=Av[:], in_=Av[:], func=AF.Sqrt, bias=epsb[:])
            P.I("dve", "reciprocal", [Av], [Av], out=Av[:], in_=Av[:])
            P.I("dve", "scalar_tensor_tensor", [mean, Av], [Bv], out=Bv[:], in0=mean[:], scalar=-1.0, in1=Av[:],
                op0=ALU.mult, op1=ALU.mult)
            for k in range(8):
                t_ = tk[k % 2]
                P.I("dve", "tensor_tensor", [ucin, Av], [t_], out=t_[:], in0=ucin[:, k, :], in1=Av[:], op=ALU.mult)
                P.I("dve", "tensor_tensor", [t_, Bv], [t_], out=t_[:], in0=t_[:], in1=Bv[:], op=ALU.add)
                P.I("act", "activation", [t_, sp_["clg"], sp_["clb"]], [ucb], partial=(k > 0), out=ucb[:, k, :], in_=t_[:], func=AF.Silu,
                    scale=sp_["clg"][:, k:k + 1], bias=sp_["clb"][:, k:k + 1])

        def c_main_a(T):
            ts_ = slice(T * 512, (T + 1) * 512)
            for j in range(8):
                g_ = gin[j % 2]
                P.dma("sp", g_[:, 0, :], S_g[j, :, ts_], g_, reads=[S_g], writes=[g_])
                P.dma("sp", g_[:, 1, :], S_g[8 + j, :, ts_], g_, reads=[S_g], writes=[g_], partial=True)
                b1 = P.bank()
                for k in range(8):
                    P.mm(b1, b1[:], cwo, cwo[:, k, j * 128:(j + 1) * 128], ucb, ucb[:, k, :], k == 0, k == 7)
                b2 = P.bank()
                for k in range(16):
                    P.mm(b2, b2[:], swo, swo[:, k, j * 128:(j + 1) * 128], ynin, ynin[:, k, :], k == 0, k == 15)
                a_ = ta[j % 2]
                b_ = tb[j % 2]
                P.I("dve", "tensor_tensor", [b1, g_], [a_], out=a_[:], in0=b1[:], in1=g_[:, 0, :], op=ALU.mult)
                P.I("dve", "tensor_tensor", [b2, g_], [b_], out=b_[:], in0=b2[:], in1=g_[:, 1, :], op=ALU.mult)
                P.I("dve", "tensor_tensor", [a_, b_], [hT], partial=(j > 0), out=hT[:, j, :], in0=a_[:], in1=b_[:], op=ALU.add)

        def c_main_b(T):
            for sub in range(4):
                tok0 = T * 512 + sub * 128
                xr = xres[sub % 2]
                P.dma("sp", xr[:], x_src[tok0:tok0 + 128, :], xr, reads=[x_src], writes=[xr])
                for hf in range(2):
                    bk = P.bank()
                    for k in range(8):
                        P.mm(bk, bk[:], hT, hT[:, k, sub * 128:(sub + 1) * 128], wo, wo[:, k, hf * 512:(hf + 1) * 512], k == 0, k == 7)
                    P.I("dve", "scalar_tensor_tensor", [xr, bk], [vv4], partial=True, out=vv4[:, sub, hf * 512:(hf + 1) * 512],
                        in0=xr[:, hf * 512:(hf + 1) * 512], scalar=ALPHA, in1=bk[:], op0=ALU.mult, op1=ALU.add)
                for i_ in range(2):
                    P.I("dve", "bn_stats", [vv4], [st6], partial=(i_ > 0), out=st6[:, i_, :], in_=vv4[:, sub, i_ * 512:(i_ + 1) * 512])
                P.I("dve", "bn_aggr", [st6], [mv4], partial=True, out=mv4[:, sub, :], in_=st6[:])
            P.I("act", "activation", [mv4, epsb], [rs4c], out=rs4c[:], in_=mv4[:, :, 1], func=AF.Ln, bias=epsb[:])
            P.I("act", "activation", [rs4c], [rs4c], out=rs4c[:], in_=rs4c[:], func=AF.Exp, scale=-0.5)
            P.I("dve", "scalar_tensor_tensor", [mv4, rs4c], [nm4], out=nm4[:], in0=mv4[:, :, 0], scalar=-1.0, in1=rs4c[:],
                op0=ALU.mult, op1=ALU.mult)
            for sub in range(4):
                tok0 = T * 512 + sub * 128
                o = xn[sub % 2]
                P.I("act", "activation", [vv4, rs4c, nm4], [o], out=o[:], in_=vv4[:, sub, :], func=AF.Identity,
                    scale=rs4c[:, sub:sub + 1], bias=nm4[:, sub:sub + 1])
                P.I("dve", "tensor_tensor", [o, lng], [o], out=o[:], in0=o[:], in1=lng[:], op=ALU.mult)
                P.I("dve", "tensor_tensor", [o, lnb], [o], out=o[:], in0=o[:], in1=lnb[:], op=ALU.add)
                P.dma("sp", S_x1[tok0:tok0 + 128, :], o[:], o, reads=[o], writes=[S_x1], partial=True)

        c_prep(0)
        for T in range(4):
            c_main_a(T)
            if T + 1 < 4:
                c_prep(T + 1)
            c_main_b(T)
        P.release(mC)
        if STOP == (l, "C"):
            P.barrier()
            P.emit()
            return P

        mD = P.mark()
        moe = (l % 2 == 1) and not FORCE_DENSE
        x1T = P.sb("x1T", [128, 8, L], BF16)
        P.dma("sp", lng[:], w["lfg"], lng, writes=[lng])
        P.dma("sp", lnb[:], w["lfb"], lnb, writes=[lnb])
        xin = [P.sb(f"x1in{i}", [128, 1024], F32) for i in range(2)]
        xinb = [P.sb(f"x1inb{i}", [128, 1024], BF16) for i in range(2)]
        for tt in range(NT):
            xtb = xinb[tt % 2]
            P.dma("pool", xtb[:], S_x1[tt * 128:(tt + 1) * 128, :], xtb, reads=[S_x1], writes=[xtb])
            bk = P.bank()
            bkv = bk.t.bitcast(BF16)
            for k in range(8):
                P.tr(bk, bkv[:, k * 128:(k + 1) * 128], xtb, xtb[:, k * 128:(k + 1) * 128], identb)
            P.I("act", "copy", [bk], [x1T], partial=True, out=x1T[:, :, tt * 128:(tt + 1) * 128],
                in_=bkv[:, :].rearrange("p (j t) -> p j t", t=128))
        if STOP == (l, "R"):
            P.barrier()
            P.emit()
            return P
        nexp = (NEXP_RUN if moe else 1)
        nfb = 28 if moe else 22
        FG = 7
        gsz = [7, 7, 7, 7] if moe else [6, 6, 5, 5]
        ngrp = 4
        wgu_d = wgu1 if moe else wgu0
        wdn_d = wd1 if moe else wd0
        wg = [P.sb(f"wgu{i}", [128, 8, 256], BF16) for i in range(3)]
        wdn = [P.sb(f"wdn{i}", [128, FG, 1024], BF16) for i in range(2)]
        hTt = [P.sb(f"hTt{i}", [128, FG, 1024], BF16) for i in range(2)]
        yacc = P.sb("yacc", [128, 8, 1024], F32)
        sgt = [P.sb(f"sgt{i}", [128, 512], F32) for i in range(2)]
        vv = P.sb("vv2", [128, 1024], F32)
        xo_ = [P.sb(f"xout{i}", [128, 1024], F32) for i in range(2)]
        st6 = P.sb("st6b", [128, 2, 6], F32)
        mv = P.sb("mvb", [128, 2], F32)
        rstd = P.sb("rstdb", [128, 1], F32)
        nmr = P.sb("nmrb", [128, 1], F32)
        mv8 = P.sb("mv8", [128, 8, 2], F32)
        rs8 = P.sb("rs8", [128, 8], F32)
        nm8 = P.sb("nm8", [128, 8], F32)
        pend_router = []
        if moe:
            lg = P.sb("lg", [128, 16, 8], F32)
            m8 = P.sb("m8", [128, 16, 8], F32)
            ex = P.sb("ex", [128, 16, 8], F32)
            nm1 = P.sb("nm1", [128, 16], F32)
            den = P.sb("den", [128, 16], F32)
            P.dma("sp", yacc[:], wr_d, yacc, writes=[yacc])
            P.I("dve", "memset", [], [lg], ap=lg[:], constant=0.0)

            def router_tile(tt):
                xt = xin[tt % 2]
                P.dma("sp", xt[:], S_x1[tt * 128:(tt + 1) * 128, :], xt, reads=[S_x1], writes=[xt])
                for e_ in range(NEXP):
                    P.I("dve", "scalar_tensor_tensor", [xt, yacc], [vv, lg], partial=True, out=vv[:], in0=xt[:], scalar=1.0,
                        in1=yacc[:, e_, :], op0=ALU.mult, op1=ALU.mult, accum_out=lg[:, tt, e_:e_ + 1])

            def router_math():
                for tt in range(NT):
                    P.I("dve", "max", [lg], [m8], partial=(tt > 0), out=m8[:, tt, :], in_=lg[:, tt, :])
                P.I("dve", "tensor_scalar", [m8], [nm1], out=nm1[:], in0=m8[:, :, 0], scalar1=-1.0, scalar2=None, op0=ALU.mult)
                for tt in range(NT):
                    P.I("act", "activation", [lg, nm1], [ex], partial=(tt > 0), out=ex[:, tt, :], in_=lg[:, tt, :], func=AF.Exp,
                        bias=nm1[:, tt:tt + 1])
                    P.I("dve", "tensor_scalar", [lg, m8], [cwt], partial=(tt > 0), out=cwt[:, tt, :], in0=lg[:, tt, :],
                        scalar1=m8[:, tt, 1:2], scalar2=None, op0=ALU.is_ge)
                P.I("dve", "tensor_tensor", [cwt, ex], [cwt], out=cwt[:], in0=cwt[:], in1=ex[:], op=ALU.mult)
                P.I("dve", "tensor_tensor", [m8], [den], out=den[:], in0=m8[:, :, 1], in1=m8[:, :, 0], op=ALU.subtract)
                P.I("act", "activation", [den], [den], out=den[:], in_=den[:], func=AF.Exp)
                P.I("dve", "tensor_scalar", [den], [den], out=den[:], in0=den[:], scalar1=1.0, scalar2=None, op0=ALU.add)
                P.I("dve", "reciprocal", [den], [den], out=den[:], in_=den[:])
                P.I("dve", "tensor_tensor", [cwt, den], [cwt], out=cwt[:], in0=cwt[:],
                    in1=den[:].rearrange("p (t o) -> p t o", o=1).broadcast_to([128, 16, 8]), op=ALU.mult)

            for tt in range(NT):
                pend_router.append((router_tile, (tt,)))
            pend_router.append((router_math, ()))

        def layer_norm_rows2(v, o):
            for i_ in range(2):
                P.I("dve", "bn_stats", [v], [st6], partial=(i_ > 0), out=st6[:, i_, :], in_=v[:, i_ * 512:(i_ + 1) * 512])
            P.I("dve", "bn_aggr", [st6], [mv], out=mv[:], in_=st6[:])
            P.I("act", "activation", [mv, epsb], [rstd], out=rstd[:], in_=mv[:, 1:2], func=AF.Sqrt, bias=epsb[:])
            P.I("dve", "reciprocal", [rstd], [rstd], out=rstd[:], in_=rstd[:])
            P.I("dve", "scalar_tensor_tensor", [mv, rstd], [nmr], out=nmr[:], in0=mv[:, 0:1], scalar=-1.0, in1=rstd[:],
                op0=ALU.mult, op1=ALU.mult)
            P.I("act", "activation", [v, rstd, nmr], [o], out=o[:], in_=v[:], func=AF.Identity, scale=rstd[:], bias=nmr[:])
            P.I("dve", "tensor_tensor", [o, lng], [o], out=o[:], in0=o[:], in1=lng[:], op=ALU.mult)
            P.I("dve", "tensor_tensor", [o, lnb], [o], out=o[:], in0=o[:], in1=lnb[:], op=ALU.add)

        wq = []
        for T2 in range(2):
            for e in range(nexp):
                for f in range(nfb):
                    wq.append((e, f))
        nload = [0]

        def issue_load():
            i = nload[0]
            if i >= len(wq):
                return
            e, f = wq[i]
            slot = wg[i % 3]
            P.dma("pool", flat(slot[:]), wgu_d[e, f].rearrange("p k n -> p (k n)"), slot, writes=[slot])
            nload[0] += 1

        issue_load()
        issue_load()
        wi = 0
        gi = 0
        pend_tail = []
        for T2 in range(2):
            first_acc = True
            for e in range(nexp):
                for gq in range(ngrp):
                    wdb = wdn[gi % 2]
                    hTb = hTt[gi % 2]
                    gi += 1
                    FGq = gsz[gq]
                    r0 = sum(gsz[:gq]) * 128
                    P.dma("pool", wdb[:, 0:FGq, :], wdn_d[e, r0:r0 + FGq * 128, :].rearrange("(f p) n -> p f n", p=128), wdb, writes=[wdb])
                    for fl in range(FGq):
                        issue_load()
                        slot = wg[wi % 3]
                        wi += 1
                        for _ in range(3):
                            if pend_router:
                                fn_, a_ = pend_router.pop(0)
                                fn_(*a_)
                        for _ in range(3):
                            if pend_tail:
                                fn_, a_ = pend_tail.pop(0)
                                fn_(*a_)
                        bgs = [P.bank(), P.bank()]
                        bus = [P.bank(), P.bank()]
                        for k in range(8):
                            for s5 in range(2):
                                c0 = T2 * 1024 + s5 * 512
                                P.mm(bgs[s5], bgs[s5][:], slot, slot[:, k, 0:128], x1T, x1T[:, k, c0:c0 + 512], k == 0, k == 7)
                        for k in range(8):
                            for s5 in range(2):
                                c0 = T2 * 1024 + s5 * 512
                                P.mm(bus[s5], bus[s5][:], slot, slot[:, k, 128:256], x1T, x1T[:, k, c0:c0 + 512], k == 0, k == 7)
                        for s5 in range(2):
                            sg_ = sgt[s5]
                            P.I("act", "activation", [bgs[s5]], [sg_], out=sg_[:], in_=bgs[s5][:], func=AF.Silu)
                            P.I("dve", "tensor_tensor", [bus[s5], sg_], [hTb], partial=True, out=hTb[:, fl, s5 * 512:(s5 + 1) * 512],
                                in0=bus[s5][:], in1=sg_[:], op=ALU.mult)
                    for sub in range(8):
                        bks = [P.bank(), P.bank()]
                        for fl in range(FGq):
                            for hf in range(2):
                                P.mm(bks[hf], bks[hf][:], hTb, hTb[:, fl, sub * 128:(sub + 1) * 128], wdb, wdb[:, fl, hf * 512:(hf + 1) * 512],
                                     fl == 0, fl == FGq - 1)
                        for hf in range(2):
                            bk = bks[hf]
                            ya = yacc[:, sub, hf * 512:(hf + 1) * 512]
                            tt = T2 * 8 + sub
                            if moe:
                                csc = cwt[:, tt, e:e + 1]
                                if first_acc:
                                    P.I("dve", "tensor_scalar", [bk, cwt], [yacc], partial=True, out=ya, in0=bk[:], scalar1=csc, scalar2=None,
                                        op0=ALU.mult)
                                else:
                                    P.I("dve", "scalar_tensor_tensor", [bk, cwt, yacc], [yacc], partial=True, out=ya, in0=bk[:], scalar=csc,
                                        in1=ya, op0=ALU.mult, op1=ALU.add)
                            else:
                                if first_acc:
                                    P.I("dve", "tensor_copy", [bk], [yacc], partial=True, out=ya, in_=bk[:])
                                else:
                                    P.I("dve", "tensor_tensor", [bk, yacc], [yacc], partial=True, out=ya, in0=bk[:], in1=ya, op=ALU.add)
                    first_acc = False
            steps = []

            def stA(sub, T2=T2):
                tok0 = T2 * 1024 + sub * 128
                xt = xin[sub % 2]
                P.dma("sp", xt[:], S_x1[tok0:tok0 + 128, :], xt, reads=[S_x1], writes=[xt])
                P.I("dve", "scalar_tensor_tensor", [xt, yacc], [yacc], partial=True, out=yacc[:, sub, :], in0=xt[:], scalar=ALPHA,
                    in1=yacc[:, sub, :], op0=ALU.mult, op1=ALU.add)
                for i_ in range(2):
                    P.I("dve", "bn_stats", [yacc], [st6], partial=(i_ > 0), out=st6[:, i_, :], in_=yacc[:, sub, i_ * 512:(i_ + 1) * 512])
                P.I("dve", "bn_aggr", [st6], [mv8], partial=True, out=mv8[:, sub, :], in_=st6[:])

            def stB():
                P.I("act", "activation", [mv8, epsb], [rs8], out=rs8[:], in_=mv8[:, :, 1], func=AF.Sqrt, bias=epsb[:])
                P.I("dve", "reciprocal", [rs8], [rs8], out=rs8[:], in_=rs8[:])
                P.I("dve", "scalar_tensor_tensor", [mv8, rs8], [nm8], out=nm8[:], in0=mv8[:, :, 0], scalar=-1.0, in1=rs8[:],
                    op0=ALU.mult, op1=ALU.mult)

            def stC(sub, T2=T2):
                tok0 = T2 * 1024 + sub * 128
                o = xo_[sub % 2]
                P.I("act", "activation", [yacc, rs8, nm8], [o], out=o[:], in_=yacc[:, sub, :], func=AF.Identity,
                    scale=rs8[:, sub:sub + 1], bias=nm8[:, sub:sub + 1])
                P.I("dve", "tensor_tensor", [o, lng], [o], out=o[:], in0=o[:], in1=lng[:], op=ALU.mult)
                P.I("dve", "tensor_tensor", [o, lnb], [o], out=o[:], in0=o[:], in1=lnb[:], op=ALU.add)
                P.dma("sp", x_dst[tok0:tok0 + 128, :], o[:], o, reads=[o], writes=[x_dst], partial=True)

            for sub in range(8):
                steps.append((stA, (sub,)))
            steps.append((stB, ()))
            for sub in range(8):
                steps.append((stC, (sub,)))
            if T2 == 0:
                pend_tail.extend(steps)
            else:
                for fn_, a_ in steps:
                    fn_(*a_)
        P.release(mD)
        x_src = x_dst

    P.barrier()
    P.emit()
    return P


def _prep(inputs):
    f32 = np.float32
    sh = {}
    sh["c_identf"] = np.eye(128, dtype=f32)
    t = np.arange(128)
    sh["c_U"] = (t[:, None] <= t[None, :]).astype(f32)
    sh["c_Ls"] = (t[:, None] > t[None, :]).astype(f32)
    sh["c_ones"] = np.ones((128, 128), f32)
    sh["c_idst"] = np.concatenate([np.eye(64, dtype=f32), np.eye(64, dtype=f32)], axis=0)

    def pk(wm):
        n = wm.shape[1]
        return np.ascontiguousarray(wm.reshape(8, 128, n).transpose(1, 0, 2))

    def rep(v):
        return np.ascontiguousarray(np.broadcast_to(np.asarray(v, f32)[None, :], (128, v.shape[0])))

    def pp(v, nb):
        return np.ascontiguousarray(np.asarray(v, f32).reshape(nb, 128).T)

    for l in range(2):
        win = np.asarray(inputs["mix_w_in"][l], f32)
        cv, cg, z, xbc, dt, gates = np.split(win, np.cumsum([1024, 1024, 2048, 3072, 32])[:5].tolist(), axis=1)
        cols = []
        for i in range(8):
            cols.append(cg[:, i * 128:(i + 1) * 128])
            cols.append(cv[:, i * 128:(i + 1) * 128])
        cols.append(xbc)
        cols.append(gates)
        wa = np.concatenate(cols, axis=1)
        sh[f"win_a{l}"] = np.ascontiguousarray(pk(wa).reshape(128, 8, 14, 512).transpose(2, 0, 1, 3))
        sh[f"win_z{l}"] = np.ascontiguousarray(pk(z).reshape(128, 8, 4, 512).transpose(2, 0, 1, 3))
        sh[f"win_dt{l}"] = pk(dt)
        sh[f"cdw{l}"] = np.ascontiguousarray(np.asarray(inputs["conv_dw_w"][l], f32).reshape(31, 8, 128).transpose(2, 1, 0))
        sh[f"cdb{l}"] = pp(inputs["conv_dw_b"][l], 8)
        sh[f"clg{l}"] = pp(inputs["conv_ln_g"][l], 8)
        sh[f"clb{l}"] = pp(inputs["conv_ln_b"][l], 8)
        sh[f"scw{l}"] = np.ascontiguousarray(np.asarray(inputs["ssm_conv_w"][l], f32).reshape(4, 24, 128).transpose(2, 1, 0))
        sh[f"scb{l}"] = pp(inputs["ssm_conv_b"][l], 24)
        sh[f"dtb{l}"] = rep(inputs["ssm_dt_bias"][l])
        sh[f"alog{l}"] = rep(inputs["ssm_a_log"][l])
        dd = np.asarray(inputs["ssm_d"][l], f32)
        dppv = np.zeros((128, 16), f32)
        dppv[:64, :] = dd[0::2][None, :]
        dppv[64:, :] = dd[1::2][None, :]
        sh[f"dpp{l}"] = dppv
        sh[f"nw{l}"] = pp(inputs["ssm_norm_w"][l], 16)
        sh[f"cwo{l}"] = pk(np.asarray(inputs["conv_w_out"][l], f32))
        sw = np.asarray(inputs["ssm_w_out"][l], f32)
        sh[f"swo{l}"] = np.ascontiguousarray(sw.reshape(16, 128, 1024).transpose(1, 0, 2))
        sh[f"wo{l}"] = pk(np.asarray(inputs["mix_w_out"][l], f32))
        sh[f"lmg{l}"] = rep(inputs["ln_mix_g"][l])
        sh[f"lmb{l}"] = rep(inputs["ln_mix_b"][l])
        sh[f"lfg{l}"] = rep(inputs["ln_ffn_g"][l])
        sh[f"lfb{l}"] = rep(inputs["ln_ffn_b"][l])

    def gu(wgm, wum, nfb):
        a = pk(wgm).reshape(128, 8, nfb, 128)
        b = pk(wum).reshape(128, 8, nfb, 128)
        return np.ascontiguousarray(np.concatenate([a, b], axis=3).transpose(2, 0, 1, 3))

    sh["wgu0"] = gu(np.asarray(inputs["ffn_w_gate"][0], f32), np.asarray(inputs["ffn_w_up"][0], f32), 22)[None]
    sh["wd0"] = np.ascontiguousarray(np.asarray(inputs["ffn_w_down"], f32))
    sh["wgu1"] = np.stack([gu(np.asarray(inputs["moe_w_gate"][0, e], f32), np.asarray(inputs["moe_w_up"][0, e], f32), 28)
                           for e in range(NEXP)], axis=0)
    sh["wd1"] = np.ascontiguousarray(np.asarray(inputs["moe_w_down"][0], f32))
    sh["wr"] = np.ascontiguousarray(np.broadcast_to(np.asarray(inputs["moe_router"][0], f32).T[None], (128, 8, 1024)))
    return sh


def kernel(**inputs):
    x = np.asarray(inputs["x"], np.float32)
    sh = _prep(inputs)
    nc = bass.Bass("TRN2", target_bir_lowering=False)
    build(nc)
    in_maps = []
    for c in range(8):
        m = dict(sh)
        m["x"] = np.ascontiguousarray(x[c])
        in_maps.append(m)
    res = run_bass_kernel_spmd(nc, in_maps, core_ids=list(range(8)))
    return np.stack([np.asarray(r["out"], np.float32) for r in res.results], axis=0)
```
